# Optimizing a Trainium2 kernel written in Bass

```python
import jax, jax.numpy as jnp
from jax import lax
import numpy as np

D_MODEL = 1024
BATCH = 4
SEQ = 4096
DEPTH = 2
DEC_BATCH = 8
DEC_SEQ = 4096
PAST_LEN = 128

HEAD_DIM = 64
N_HEADS = D_MODEL // HEAD_DIM
GRID_W = 64
A_HEADS = (3 * N_HEADS) // 8
A_KV = 2
A_WINDOW = 128
A_BLOCK = 128
B_HEADS = N_HEADS // 4
B_MAX_ROWS = 8
B_COLS = 16
C_HEADS = N_HEADS - A_HEADS - B_HEADS
C_KV = 2
C_BLOCK = 128
ROPE_THETA = 10000.0
N_GROUPS = 4
EXPERTS_PER_GROUP = 4
N_EXPERTS = N_GROUPS * EXPERTS_PER_GROUP
TOP_K = 2
D_EXPERT = D_MODEL // 2
EPS = 1e-6
NEG = -1e30

A_WIDTH = A_HEADS * HEAD_DIM
B_WIDTH = B_HEADS * HEAD_DIM
C_WIDTH = C_HEADS * HEAD_DIM
IN_SIZES = [A_WIDTH, A_KV * HEAD_DIM, A_KV * HEAD_DIM,
            B_WIDTH, B_WIDTH, B_WIDTH,
            C_WIDTH, C_KV * HEAD_DIM, C_KV * HEAD_DIM]
IN_COLS = sum(IN_SIZES)
IN_SPLITS = [sum(IN_SIZES[:i + 1]) for i in range(len(IN_SIZES) - 1)]

kernel_name = 'hybrid_parallel_head_encoder'


def rms_norm(x, g):
    xf = x.astype(jnp.float32)
    y = xf * lax.rsqrt(jnp.mean(xf * xf, axis=-1, keepdims=True) + EPS)
    return (y * g.astype(jnp.float32)).astype(x.dtype)


def window_attention(q, k, v, sink):
    bsz, t, h, dh = q.shape
    g = h // A_KV
    nb = t // A_BLOCK
    pad = ((0, 0), (A_BLOCK, A_BLOCK), (0, 0), (0, 0))
    kp = jnp.pad(k, pad).reshape(bsz, nb + 2, A_BLOCK, A_KV, dh)
    vp = jnp.pad(v, pad).reshape(bsz, nb + 2, A_BLOCK, A_KV, dh)
    kb = jnp.concatenate([kp[:, :-2], kp[:, 1:-1], kp[:, 2:]], axis=2)
    vb = jnp.concatenate([vp[:, :-2], vp[:, 1:-1], vp[:, 2:]], axis=2)
    qb = q.reshape(bsz, nb, A_BLOCK, A_KV, g, dh)
    s = jnp.einsum('bnqhgd,bnkhd->bnhgqk', qb, kb).astype(jnp.float32) * (dh ** -0.5)
    i = jnp.arange(A_BLOCK)
    j = jnp.arange(3 * A_BLOCK)
    dist = (A_BLOCK + i[:, None] - j[None, :]).astype(jnp.float32)
    kpos = (jnp.arange(nb)[:, None] - 1) * A_BLOCK + j[None, :]
    mask = (jnp.abs(dist)[None] <= A_WINDOW) & ((kpos >= 0) & (kpos < t))[:, None, :]
    slopes = jnp.asarray(np.array([2.0 ** (-8.0 * (n + 1) / A_HEADS) for n in range(A_HEADS)], np.float32))
    slopes = slopes.reshape(A_KV, g)
    s = s - slopes[:, :, None, None] * jnp.abs(dist)
    s = jnp.where(mask[None, :, None, None], s, NEG)
    sink_col = jnp.broadcast_to(sink.astype(jnp.float32).reshape(A_KV, g)[:, :, None, None],
                                s.shape[:-1] + (1,))
    p = jax.nn.softmax(jnp.concatenate([s, sink_col], axis=-1), axis=-1)[..., :-1]
    o = jnp.einsum('bnhgqk,bnkhd->bnqhgd', p.astype(v.dtype), vb)
    return o.reshape(bsz, t, h, dh)


def neighborhood_attention(q, k, v, rpb):
    bsz, t, h, dh = q.shape
    rows = t // GRID_W
    kh = min(B_MAX_ROWS, rows)
    r = jnp.arange(rows)
    r0 = jnp.clip(r - kh // 2, 0, rows - kh)
    key_rows = r0[:, None] + jnp.arange(kh)[None, :]
    kg = k.reshape(bsz, rows, GRID_W, h, dh)[:, key_rows]
    vg = v.reshape(bsz, rows, GRID_W, h, dh)[:, key_rows]
    qg = q.reshape(bsz, rows, GRID_W, h, dh)
    s = jnp.einsum('brqhd,brkwhd->brhqkw', qg, kg).astype(jnp.float32) * (dh ** -0.5)
    c = jnp.arange(GRID_W)
    c0 = jnp.clip(c - B_COLS // 2, 0, GRID_W - B_COLS)
    col_ok = (c[None, :] >= c0[:, None]) & (c[None, :] < c0[:, None] + B_COLS)
    dr = key_rows - r[:, None]
    dc = jnp.clip(c[None, :] - c[:, None], -(B_COLS - 1), B_COLS - 1)
    bias = rpb.astype(jnp.float32)[:, dr[:, :, None, None] + (B_MAX_ROWS - 1),
                                   dc[None, None] + (B_COLS - 1)]
    s = s + bias.transpose(1, 0, 3, 2, 4)[None]
    s = jnp.where(col_ok[:, None, :], s, NEG)
    p = jax.nn.softmax(s.reshape(bsz, rows, h, GRID_W, kh * GRID_W), axis=-1)
    p = p.reshape(bsz, rows, h, GRID_W, kh, GRID_W)
    o = jnp.einsum('brhqkw,brkwhd->brqhd', p.astype(v.dtype), vg)
    return o.reshape(bsz, t, h, dh)


def axial_rope(x, row, col):
    dh = x.shape[-1]
    half = dh // 2
    nf = half // 2
    inv = ROPE_THETA ** (-jnp.arange(nf, dtype=jnp.float32) / nf)
    xf = x.astype(jnp.float32)

    def rot(xp, pos):
        ang = pos.astype(jnp.float32)[:, None] * inv[None, :]
        cos = jnp.cos(ang)[None, :, None, :]
        sin = jnp.sin(ang)[None, :, None, :]
        x1, x2 = xp[..., :nf], xp[..., nf:]
        return jnp.concatenate([x1 * cos - x2 * sin, x1 * sin + x2 * cos], axis=-1)

    return jnp.concatenate([rot(xf[..., :half], row), rot(xf[..., half:], col)], axis=-1).astype(x.dtype)


def global_attention(q, k, v):
    bsz, t, h, dh = q.shape
    g = h // C_KV
    nb = t // C_BLOCK
    qb = q.reshape(bsz, nb, C_BLOCK, C_KV, g, dh).transpose(1, 0, 2, 3, 4, 5)

    def one_block(qblk):
        s = jnp.einsum('bqhgd,bkhd->bhgqk', qblk, k).astype(jnp.float32) * (dh ** -0.5)
        p = jax.nn.softmax(s, axis=-1)
        return jnp.einsum('bhgqk,bkhd->bqhgd', p.astype(v.dtype), v)

    o = lax.map(one_block, qb)
    return o.transpose(1, 0, 2, 3, 4, 5).reshape(bsz, t, h, dh)


def hierarchical_moe(h, w_rg, b_rg, w_re, b_re, w_gate, w_up, w_down):
    bsz, t, d = h.shape
    hf = h.reshape(-1, d)
    n = hf.shape[0]
    g_logits = (hf @ w_rg).astype(jnp.float32) + b_rg.astype(jnp.float32)
    g_prob = jax.nn.softmax(g_logits, axis=-1)
    grp = jnp.argmax(g_logits, axis=-1)
    p_grp = jnp.take_along_axis(g_prob, grp[:, None], axis=-1)
    e_logits = ((hf @ w_re).astype(jnp.float32) + b_re.astype(jnp.float32)).reshape(n, N_GROUPS, EXPERTS_PER_GROUP)
    e_sel = jnp.take_along_axis(e_logits, grp[:, None, None], axis=1)[:, 0]
    top_v, top_i = lax.top_k(e_sel, TOP_K)
    w = jax.nn.softmax(top_v, axis=-1) * p_grp
    expert_id = grp[:, None] * EXPERTS_PER_GROUP + top_i
    gates = jnp.sum(jax.nn.one_hot(expert_id, N_EXPERTS, dtype=jnp.float32) * w[..., None], axis=1)
    gates = gates.astype(h.dtype)
    y = jnp.zeros_like(hf)
    for e in range(N_EXPERTS):
        a = jax.nn.silu(hf @ w_gate[e]) * (hf @ w_up[e])
        y = y + gates[:, e:e + 1] * (a @ w_down[e])
    return y.reshape(bsz, t, d)


def encoder_layer(x, ln1, w_in, qk_gain, sink, rpb, out_gain, w_out, ln2,
                  w_rg, b_rg, w_re, b_re, w_gate, w_up, w_down):
    bsz, t, _ = x.shape
    h = rms_norm(x, ln1)
    proj = jnp.einsum('btd,dc->btc', h, w_in)
    qa, ka, va, qb, kb, vb, qc, kc, vc = jnp.split(proj, IN_SPLITS, axis=-1)

    def heads(z):
        return z.reshape(bsz, t, -1, HEAD_DIM)

    pos = jnp.arange(t)
    row = pos // GRID_W
    col = pos % GRID_W
    o_a = window_attention(rms_norm(heads(qa), qk_gain[0, 0]), rms_norm(heads(ka), qk_gain[0, 1]),
                           heads(va), sink)
    o_b = neighborhood_attention(rms_norm(heads(qb), qk_gain[1, 0]), rms_norm(heads(kb), qk_gain[1, 1]),
                                 heads(vb), rpb)
    o_c = global_attention(axial_rope(rms_norm(heads(qc), qk_gain[2, 0]), row, col),
                           axial_rope(rms_norm(heads(kc), qk_gain[2, 1]), row, col),
                           heads(vc))
    o = jnp.concatenate([
        rms_norm(o_a.reshape(bsz, t, A_WIDTH), out_gain[:A_WIDTH]),
        rms_norm(o_b.reshape(bsz, t, B_WIDTH), out_gain[A_WIDTH:A_WIDTH + B_WIDTH]),
        rms_norm(o_c.reshape(bsz, t, C_WIDTH), out_gain[A_WIDTH + B_WIDTH:]),
    ], axis=-1)
    x = x + jnp.einsum('btc,cd->btd', o, w_out)
    x = x + hierarchical_moe(rms_norm(x, ln2), w_rg, b_rg, w_re, b_re, w_gate, w_up, w_down)
    return x


def run_trunk(x, ln1, w_in, qk_gain, sink, rpb, out_gain, w_out, ln2,
              w_rg, b_rg, w_re, b_re, w_gate, w_up, w_down):
    for l in range(DEPTH):
        x = encoder_layer(x, ln1[l], w_in[l], qk_gain[l], sink[l], rpb[l], out_gain[l], w_out[l], ln2[l],
                          w_rg[l], b_rg[l], w_re[l], b_re[l], w_gate[l], w_up[l], w_down[l])
    return x


def setup_inputs(seed: int = 0) -> dict:
    key = jax.random.key(seed)
    ks = jax.random.split(key, 20)
    f32 = jnp.float32
    nrm = lambda k, shape, scale: jax.random.normal(k, shape, f32) * scale
    return {
        'x_prompt': nrm(ks[0], (BATCH, SEQ, D_MODEL), 1.0),
        'x_sample': nrm(ks[1], (DEC_BATCH, DEC_SEQ, D_MODEL), 1.0),
        'ln1': 1.0 + nrm(ks[2], (DEPTH, D_MODEL), 0.05),
        'w_in': nrm(ks[3], (DEPTH, D_MODEL, IN_COLS), D_MODEL ** -0.5),
        'qk_gain': 1.0 + nrm(ks[4], (DEPTH, 3, 2, HEAD_DIM), 0.05),
        'sink': nrm(ks[5], (DEPTH, A_HEADS), 0.5),
        'rpb': nrm(ks[6], (DEPTH, B_HEADS, 2 * B_MAX_ROWS - 1, 2 * B_COLS - 1), 0.1),
        'out_gain': 1.0 + nrm(ks[7], (DEPTH, D_MODEL), 0.05),
        'w_out': nrm(ks[8], (DEPTH, D_MODEL, D_MODEL), D_MODEL ** -0.5),
        'ln2': 1.0 + nrm(ks[9], (DEPTH, D_MODEL), 0.05),
        'w_router_group': nrm(ks[10], (DEPTH, D_MODEL, N_GROUPS), D_MODEL ** -0.5),
        'b_router_group': nrm(ks[11], (DEPTH, N_GROUPS), 0.01),
        'w_router_expert': nrm(ks[12], (DEPTH, D_MODEL, N_EXPERTS), D_MODEL ** -0.5),
        'b_router_expert': nrm(ks[13], (DEPTH, N_EXPERTS), 0.01),
        'w_gate': nrm(ks[14], (DEPTH, N_EXPERTS, D_MODEL, D_EXPERT), D_MODEL ** -0.5),
        'w_up': nrm(ks[15], (DEPTH, N_EXPERTS, D_MODEL, D_EXPERT), D_MODEL ** -0.5),
        'w_down': nrm(ks[16], (DEPTH, N_EXPERTS, D_EXPERT, D_MODEL), D_EXPERT ** -0.5),
    }


def reference(x_prompt, x_sample, ln1, w_in, qk_gain, sink, rpb, out_gain, w_out, ln2,
              w_router_group, b_router_group, w_router_expert, b_router_expert, w_gate, w_up, w_down):
    y_prompt = run_trunk(x_prompt, ln1, w_in, qk_gain, sink, rpb, out_gain, w_out, ln2,
                         w_router_group, b_router_group, w_router_expert, b_router_expert, w_gate, w_up, w_down)
    y_sample = run_trunk(x_sample, ln1, w_in, qk_gain, sink, rpb, out_gain, w_out, ln2,
                         w_router_group, b_router_group, w_router_expert, b_router_expert, w_gate, w_up, w_down)
    return (y_prompt, y_sample)
```

```python
import numpy as np
import ml_dtypes
from contextlib import ExitStack

import concourse.bass as bass
import concourse.mybir as mybir
from concourse.alu_op_type import AluOpType as ALU
from concourse.bass_utils import run_bass_kernel_spmd

F32 = mybir.dt.float32
BF16 = mybir.dt.bfloat16
AF = mybir.ActivationFunctionType
AX = mybir.AxisListType

T = 4096
D = 1024
NT = T // 128
DEPTH = 2
NE = 16
FE = 512
EPS = 1e-6
NEG = -1e30
NCORES = 8

ENGS = ("tensor", "vector", "scalar", "gpsimd", "sync")
import os
DBG_PARTS = os.environ.get("DBG_PARTS", "CAB")
EPOCH = 30000


_UID = [0]


class Buf:
    def __init__(self, name):
        self.name = name
        _UID[0] += 1
        self.uid = _UID[0]
        self.writers = {}
        self.readers = {}
        self.dsem = None
        self.dcount = 0
        self.w_is_pe = False


class FW:
    def __init__(self, nc, stack):
        self.nc = nc
        self.stack = stack
        self.q = {e: [] for e in ENGS}
        self.seq = {e: 0 for e in ENGS}
        self.epoch = {e: 0 for e in ENGS}
        self.sems = {e: [self._newsem(f"s_{e}_0")] for e in ENGS}
        self.waited = {e: {} for e in ENGS}
        self.nbuf = 0
        self.ndsem = 0
        self.dsem_bufs = []
        self.dpool = []

    def scope_begin(self):
        return len(self.dsem_bufs)

    def scope_end(self, i):
        for sb in self.dsem_bufs[i:]:
            self.dpool.append((sb.dsem, sb.dcount))
            sb.dsem = None
        del self.dsem_bufs[i:]

    def _newsem(self, name):
        return self.stack.enter_context(self.nc.semaphore(name))

    def buf(self, name=None):
        self.nbuf += 1
        return Buf(name or f"b{self.nbuf}")

    def _wait(self, eng, dep):
        if dep[0] == "e":
            _, e2, ep, n = dep
            key = ("e", e2)
            val = (ep, n)
            sem = self.sems[e2][ep]
        else:
            _, sem, n, key = dep
            val = (0, n)
        if self.waited[eng].get(key, (-1, 0)) >= val:
            return
        self.waited[eng][key] = val
        self.q[eng].append(lambda e, sem=sem, n=n: e.wait_ge(sem, n))

    def _deps(self, eng, reads, writes, pe_acc=False):
        for b in reads:
            for w in list(b.writers.values()):
                self._wait(eng, w)
        for b in writes:
            if not (pe_acc and b.w_is_pe and eng == "tensor"):
                for w in list(b.writers.values()):
                    self._wait(eng, w)
            for r in list(b.readers.values()):
                self._wait(eng, r)

    def _tick(self, eng):
        if self.seq[eng] >= EPOCH:
            self.epoch[eng] += 1
            self.seq[eng] = 0
            self.sems[eng].append(self._newsem(f"s_{eng}_{self.epoch[eng]}"))
        self.seq[eng] += 1
        ep = self.epoch[eng]
        return ("e", eng, ep, self.seq[eng]), self.sems[eng][ep]

    def _record(self, key, dep, reads, writes, part, is_pe):
        for b in writes:
            if part:
                b.writers[key] = dep
            else:
                b.writers = {key: dep}
                b.readers = {}
            b.w_is_pe = is_pe
        for b in reads:
            b.readers[key] = dep

    def op(self, eng, fn, reads=(), writes=(), pe_acc=False, part=False):
        self._deps(eng, reads, writes, pe_acc)
        dep, sem = self._tick(eng)
        self.q[eng].append(lambda e, fn=fn, sem=sem: fn(e).then_inc(sem, 1))
        self._record(("e", eng), dep, reads, writes, part, eng == "tensor")

    def dma(self, eng, fn, reads=(), writes=(), sem=None, part=False):
        self._deps(eng, reads, writes)
        sb = sem
        if sb.dsem is None:
            if self.dpool:
                sb.dsem, sb.dcount = self.dpool.pop()
            else:
                self.ndsem += 1
                sb.dsem = self._newsem(f"d_{self.ndsem}")
            self.dsem_bufs.append(sb)
        sb.dcount += 16
        key = ("d", sb.uid)
        dep = ("d", sb.dsem, sb.dcount, key)
        hs = sb.dsem
        self.q[eng].append(lambda e, fn=fn, hs=hs: fn(e).then_inc(hs, 16))
        self._record(key, dep, reads, writes, part, False)

    def barrier(self):
        for x in ENGS:
            for y in ENGS:
                if y != x and (self.seq[y] > 0 or self.epoch[y] > 0):
                    self._wait(x, ("e", y, self.epoch[y], self.seq[y]))
            for sb in self.dsem_bufs:
                self._wait(x, ("d", sb.dsem, sb.dcount, ("d", sb.uid)))

    def emit(self):
        with self.nc.Block() as block:
            @block.tensor
            def _(e):
                for f in self.q["tensor"]:
                    f(e)

            @block.vector
            def _(e):
                for f in self.q["vector"]:
                    f(e)

            @block.scalar
            def _(e):
                for f in self.q["scalar"]:
                    f(e)

            @block.gpsimd
            def _(e):
                for f in self.q["gpsimd"]:
                    f(e)

            @block.sync
            def _(e):
                for f in self.q["sync"]:
                    f(e)


class TL:
    def __init__(self, ap, buf):
        self.ap = ap
        self.b = buf


class Arena:
    def __init__(self, nc, stack, fw, nfloat):
        self.t = stack.enter_context(nc.sbuf_tensor("arena", [128, nfloat], F32))
        self.n = nfloat
        self.off = 0
        self.fw = fw

    def mark(self):
        return self.off

    def release(self, m):
        self.off = m

    def _take(self, nf):
        nf = (nf + 7) // 8 * 8
        assert self.off + nf <= self.n, f"arena overflow {self.off}+{nf}>{self.n}"
        o = self.off
        self.off += nf
        return o

    def f32(self, n, name=None):
        o = self._take(n)
        return TL(self.t[:, o:o + n], self.fw.buf(name))

    def bf16(self, n, name=None):
        assert n % 2 == 0
        o = self._take(n // 2)
        return TL(self.t[:, o:o + n // 2].bitcast(BF16), self.fw.buf(name))


def _bf(x):
    return np.asarray(x, np.float32).astype(ml_dtypes.bfloat16).astype(np.float32)


def _b_rows(i):
    r0a = min(max(2 * i - 4, 0), 56)
    r0b = min(max(2 * i + 1 - 4, 0), 56)
    return list(range(r0a // 2, (r0b + 7) // 2 + 1))


def _b_mask(i, j):
    m = np.full((128, 128), NEG, np.float32)
    qc = np.arange(64)
    c0 = np.clip(qc - 8, 0, 48)
    kc = np.arange(64)
    colok = (kc[:, None] >= c0[None, :]) & (kc[:, None] < c0[None, :] + 16)
    for qr in range(2):
        r = 2 * i + qr
        r0 = min(max(r - 4, 0), 56)
        for kr in range(2):
            krow = 2 * j + kr
            if r0 <= krow < r0 + 8:
                blk = np.where(colok, 0.0, NEG).astype(np.float32)
                m[kr * 64:(kr + 1) * 64, qr * 64:(qr + 1) * 64] = blk
    return m


def _consts():
    c = {}
    pos = np.arange(T)
    row = (pos // 64).astype(np.float32)
    col = (pos % 64).astype(np.float32)
    inv = (10000.0 ** (-np.arange(16, dtype=np.float32) / 16)).astype(np.float32)
    ang_r = row[:, None] * inv[None, :]
    ang_c = col[:, None] * inv[None, :]
    cos = np.concatenate([np.cos(ang_r), np.cos(ang_r), np.cos(ang_c), np.cos(ang_c)], axis=1)
    sin = np.concatenate([np.sin(ang_r), np.sin(ang_r), np.sin(ang_c), np.sin(ang_c)], axis=1)
    c["rope"] = np.concatenate([cos, sin], axis=1).astype(np.float32).reshape(NT, 128, 128)
    slopes = np.array([2.0 ** (-8.0 * (n + 1) / 6) for n in range(6)], np.float32)
    k = np.arange(128)[:, None]
    q = np.arange(128)[None, :]
    ab = np.zeros((128, 6, 384), np.float32)
    for oi, o in enumerate((-1, 0, 1)):
        dist = np.abs((q - k) - 128 * o).astype(np.float32)
        for j in range(2):
            for hh in range(3):
                v = -(slopes[3 * j + hh] * dist) * 8.0
                v = np.where(dist <= 128, v, NEG).astype(np.float32)
                ab[:, oi * 2 + j, hh * 128:(hh + 1) * 128] = v
    hi = _bf(ab)
    lo = _bf(np.where(ab <= -1e29, 0.0, ab - hi))
    c["abias"] = np.stack([hi, lo], axis=0).astype(np.float32)
    pats = {}
    plist = []
    pidx = {}
    for i in range(NT):
        for j in _b_rows(i):
            m = _b_mask(i, j)
            key = m.tobytes()
            if key not in pats:
                pats[key] = len(plist)
                plist.append(m)
            pidx[(i, j)] = pats[key]
    c["maskb"] = _bf(np.stack(plist, axis=1))
    c["_pidx"] = pidx
    c["_npat"] = len(plist)
    jp = np.zeros((128, 128), np.float32)
    for qr in range(2):
        for qc in range(64):
            jp[qr * 64 + 63 - qc, qr * 64 + qc] = 1.0
    c["jperm"] = jp
    c["invn3"] = np.tile(np.array([[1 / 384.0, 1 / 256.0, 1 / 384.0]], np.float32), (128, 1))
    return c


_CONST = None


def _get_consts():
    global _CONST
    if _CONST is None:
        _CONST = _consts()
    return _CONST


def build(NSEQ=2, NL=DEPTH, stop_after=None, dbg=False):
    C = _get_consts()
    NPAT = C["_npat"]
    pidx = C["_pidx"]
    nc = bass.Bass("TRN2", target_bir_lowering=False)

    def dram(name, shape, dt=F32, kind="ExternalInput"):
        return nc.dram_tensor(name, list(shape), dt, kind=kind)

    xin = [dram(f"x{s}", [T, D]).ap() for s in range(NSEQ)]
    yout = [dram(f"y{s}", [T, D], kind="ExternalOutput").ap() for s in range(NSEQ)]
    ln1_d = dram("ln1", [DEPTH, 128, 8]).ap()
    w_in_d = dram("w_in", [DEPTH, D, 2048]).ap()
    qkg_d = dram("qk_gain", [DEPTH, 3, 2, 64])
    sink_d = dram("sink", [DEPTH, 6])
    rpb_d = dram("rpb", [DEPTH, 4, 15, 31]).ap()
    og_d = dram("out_gain", [DEPTH, 128, 8]).ap()
    w_out_d = dram("w_out", [DEPTH, D, D]).ap()
    ln2_d = dram("ln2", [DEPTH, 128, 8]).ap()
    wr_d = dram("w_router", [DEPTH, 128, 8, 20]).ap()
    gcol_d = dram("gcolh", [DEPTH, 128, 4]).ap()
    brg_d = dram("b_router_group", [DEPTH, 4])
    bre_d = dram("b_router_expert", [DEPTH, 16])
    wg_d = dram("w_gate", [DEPTH, NE, D, FE]).ap()
    wu_d = dram("w_up", [DEPTH, NE, D, FE]).ap()
    wd_d = dram("w_down", [DEPTH, NE, FE, D]).ap()
    rope_d = dram("c_rope", [NT, 128, 128]).ap()
    abias_d = dram("c_abias", [2, 128, 6, 384]).ap()
    maskb_d = dram("c_maskb", [128, NPAT, 128]).ap()
    jperm_d = dram("c_jperm", [128, 128]).ap()
    invn3_d = dram("c_invn3", [128, 3]).ap()

    skind = "ExternalOutput" if dbg else "Internal"
    x2s = [dram(f"x2s{s}", [T, D], kind=skind).ap() for s in range(NSEQ)]
    xl = [dram(f"xl{s}", [T, D], kind=skind).ap() for s in range(NSEQ)]
    qTs = [dram(f"qTs{s}", [16, 128, T], BF16, kind="Internal").ap() for s in range(NSEQ)]
    PADN = 512 + 1860 + 512
    rpad = dram("rpad", [PADN], kind="Internal")

    with ExitStack() as st:
        fw = FW(nc, st)

        def sb(name, shape, dt=F32):
            t = st.enter_context(nc.sbuf_tensor(name, list(shape), dt))
            return TL(t, fw.buf(name))

        identf = sb("identf", [128, 128], F32)
        identb = sb("identb", [128, 128], BF16)
        jperm = sb("jperm", [128, 128], F32)
        invn3 = sb("invn3", [128, 3], F32)
        abias = sb("abias", [128, 2, 6, 384], BF16)
        maskb = sb("maskb", [128, NPAT, 128], BF16)
        rbt = sb("rbt", [128, 2, 7, 512], BF16)
        g1 = sb("g1", [128, 8]); og = sb("og", [128, 8]); g2 = sb("g2", [128, 8])
        gcol = sb("gcol", [128, 4])
        gainC = sb("gainC", [128, 512])
        esA = sb("esA", [128, 6])
        wr = sb("wr", [128, 8, 20]); rb = sb("rb", [128, 20])
        ztile = sb("ztile", [4, 128])
        gtile = sb("gtile", [128, 128])
        epsb = sb("epsb", [128, 1])
        psall = st.enter_context(nc.psum_tensor("psall", [128, 4096], F32))
        banks = []
        for i in range(8):
            tl_ = TL(psall[:, i * 512:(i + 1) * 512], fw.buf(f"bank{i}"))
            tl_.col0 = i * 512
            banks.append(tl_)
        scrbuf = fw.buf("dram_scratch")
        outbuf = fw.buf("dram_out")
        arena = Arena(nc, st, fw, 43200)

        def V(tl):
            return tl.ap if isinstance(tl, TL) else tl

        def act(out, in_, func, reads, writes, part=False, **kw):
            fw.op("scalar", lambda e: e.activation(out=out, in_=in_, func=func, **kw), reads, writes, part=part)

        def rsqrt_inplace(tl, ap, n_inv):
            fw.op("vector", lambda e: e.tensor_scalar(out=ap, in0=ap, scalar1=n_inv, scalar2=EPS,
                                                       op0=ALU.mult, op1=ALU.add), [tl.b], [tl.b])
            act(ap, ap, AF.Ln, [tl.b], [tl.b])
            act(ap, ap, AF.Exp, [tl.b], [tl.b], scale=-0.5)

        fw.op("gpsimd", lambda e: e.memset(identf.ap[:], 0.0), [], [identf.b])
        fw.op("gpsimd", lambda e: e.affine_select(out=identf.ap[:], in_=identf.ap[:], pattern=[[-1, 128]],
                                                   compare_op=ALU.not_equal, fill=1.0, base=0,
                                                   channel_multiplier=1), [identf.b], [identf.b])
        fw.op("vector", lambda e: e.tensor_copy(out=identb.ap[:], in_=identf.ap[:]), [identf.b], [identb.b])
        fw.op("vector", lambda e: e.memset(ztile.ap[:], 0.0), [], [ztile.b])
        fw.dma("sync", lambda e: e.dma_start(out=jperm.ap[:], in_=jperm_d[:, :]), [], [jperm.b], sem=jperm.b)
        fw.dma("sync", lambda e: e.dma_start(out=invn3.ap[:], in_=invn3_d[:, :]), [], [invn3.b], sem=invn3.b)
        fw.dma("gpsimd", lambda e: e.dma_start(out=abias.ap[:], in_=abias_d.rearrange("t k a c -> k t a c")),
               [], [abias.b], sem=abias.b)
        fw.dma("gpsimd", lambda e: e.dma_start(out=maskb.ap[:], in_=maskb_d[:, :, :]), [], [maskb.b], sem=maskb.b)
        rpa = rpad.ap()
        fw.dma("sync", lambda e: e.dma_start(out=rpa[0:512].rearrange("(a b) -> a b", b=128), in_=ztile.ap[:]),
               [ztile.b], [scrbuf], sem=ztile.b, part=True)
        fw.dma("sync", lambda e: e.dma_start(out=rpa[2372:2884].rearrange("(a b) -> a b", b=128), in_=ztile.ap[:]),
               [ztile.b], [scrbuf], sem=ztile.b, part=True)

        def layer_setup(l):
            fw.dma("sync", lambda e: e.dma_start(out=g1.ap[:], in_=ln1_d[l]), [], [g1.b], sem=g1.b)
            fw.dma("sync", lambda e: e.dma_start(out=og.ap[:], in_=og_d[l]), [], [og.b], sem=og.b)
            fw.dma("sync", lambda e: e.dma_start(out=g2.ap[:], in_=ln2_d[l]), [], [g2.b], sem=g2.b)
            fw.dma("sync", lambda e: e.dma_start(out=gcol.ap[:], in_=gcol_d[l]), [], [gcol.b], sem=gcol.b)
            srcq = bass.AP(tensor=qkg_d, offset=((l * 3 + 2) * 2 + 0) * 64, ap=[[0, 128], [0, 6], [1, 64]])
            srck = bass.AP(tensor=qkg_d, offset=((l * 3 + 2) * 2 + 1) * 64, ap=[[0, 128], [0, 2], [1, 64]])
            fw.dma("sync", lambda e: e.dma_start(out=gainC.ap[:, 0:384].rearrange("p (h j) -> p h j", j=64), in_=srcq),
                   [], [gainC.b], sem=gainC.b, part=True)
            fw.dma("sync", lambda e: e.dma_start(out=gainC.ap[:, 384:512].rearrange("p (h j) -> p h j", j=64), in_=srck),
                   [], [gainC.b], sem=gainC.b, part=True)
            srcs = bass.AP(tensor=sink_d, offset=l * 6, ap=[[0, 128], [1, 6]])
            fw.dma("sync", lambda e: e.dma_start(out=esA.ap[:], in_=srcs), [], [esA.b], sem=esA.b)
            act(esA.ap[:], esA.ap[:], AF.Exp, [esA.b], [esA.b])
            fw.dma("sync", lambda e: e.dma_start(out=wr.ap[:], in_=wr_d[l]), [], [wr.b], sem=wr.b)
            s1 = bass.AP(tensor=brg_d, offset=l * 4, ap=[[0, 128], [1, 4]])
            s2 = bass.AP(tensor=bre_d, offset=l * 16, ap=[[0, 128], [1, 16]])
            fw.dma("sync", lambda e: e.dma_start(out=rb.ap[:, 0:4], in_=s1), [], [rb.b], sem=rb.b, part=True)
            fw.dma("sync", lambda e: e.dma_start(out=rb.ap[:, 4:20], in_=s2), [], [rb.b], sem=rb.b, part=True)
            fw.dma("sync", lambda e: e.dma_start(out=rpa[512:2372].rearrange("(a b) -> a b", b=465),
                                                   in_=rpb_d[l].rearrange("h a b -> h (a b)")),
                   [], [scrbuf], sem=scrbuf, part=True)
            fw.barrier()
            mrb = arena.mark()
            scrb = fw.scope_begin()
            gall = arena.f32(28 * 128, "gall")
            for h in range(4):
                for oi in range(7):
                    o = oi - 3
                    gi = h * 7 + oi
                    for qr in range(2):
                        src = bass.AP(tensor=rpad, offset=512 + h * 465 + (2 * o + 7 - qr) * 31 + 15 - 63,
                                      ap=[[1, 64], [31, 2], [1, 64]])
                        fw.dma("sync", lambda e, src=src, qr=qr, gi=gi: e.dma_start(
                            out=gall.ap[qr * 64:(qr + 1) * 64, gi * 128:(gi + 1) * 128].rearrange("p (a b) -> p a b", a=2), in_=src),
                            [], [gall.b], sem=gall.b, part=True)
            for h in range(4):
                for oi in range(7):
                    gi = h * 7 + oi
                    bk = banks[gi % 2]
                    fw.op("tensor", lambda e, bk=bk, gi=gi: e.matmul(bk.ap[:, 0:128], lhsT=gall.ap[:, gi * 128:(gi + 1) * 128],
                                                                     rhs=jperm.ap[:], start=True, stop=True),
                          [gall.b, jperm.b], [bk.b])
                    hi = rbt.ap[:, 0, oi, h * 128:(h + 1) * 128]
                    lo = rbt.ap[:, 1, oi, h * 128:(h + 1) * 128]
                    fw.op("vector", lambda e, hi=hi, bk=bk: e.tensor_scalar(out=hi, in0=bk.ap[:, 0:128], scalar1=8.0,
                                                                             scalar2=None, op0=ALU.mult),
                          [bk.b], [rbt.b], part=True)
                    fw.op("vector", lambda e, hi=hi, lo=lo, bk=bk: e.scalar_tensor_tensor(
                        out=lo, in0=bk.ap[:, 0:128], scalar=8.0, in1=hi, op0=ALU.mult, op1=ALU.subtract),
                        [bk.b, rbt.b], [rbt.b], part=True)
            fw.barrier()
            fw.scope_end(scrb)
            arena.release(mrb)


        def load_w_cast(dst_tl, segs, src2d, nch):
            for (dc, sap, n) in segs:
                fw.dma("gpsimd", lambda e, dc=dc, sap=sap, n=n: e.dma_start(
                    out=dst_tl.ap[:, :, dc:dc + n], in_=sap), [], [dst_tl.b], sem=dst_tl.b, part=True)

        def phase_p1(s, l, KT, VA, xsrc, wout_hook):
            m0 = arena.mark()
            sc0 = fw.scope_begin()
            Win = arena.bf16(8 * 2048, "Win"); Win.ap = Win.ap.rearrange("p (c n) -> p c n", c=8)
            wsrc = w_in_d[l]

            def rows(ap):
                return ap.rearrange("(c p) n -> p c n", p=128)

            for c in range(8):
                fw.dma("gpsimd", lambda e, c=c: e.dma_start(out=Win.ap[:, c, :], in_=wsrc[c * 128:(c + 1) * 128, :]),
                       [], [Win.b], sem=Win.b, part=True)
            for c in range(8):
                fw.op("vector", lambda e, c=c: e.tensor_scalar(out=Win.ap[:, c, :], in0=Win.ap[:, c, :],
                                                                scalar1=g1.ap[:, c:c + 1], scalar2=None, op0=ALU.mult),
                      [Win.b, g1.b], [Win.b])
            wout_hook()
            xt = [arena.f32(1024, f"xt{i}") for i in range(3)]
            rt = [arena.f32(128, f"rt{i}") for i in range(2)]
            junk = arena.f32(1536, "junk")
            sqtok = [fw.buf(f"sqtok{g}") for g in range(3)]
            junkx = arena.bf16(1024, "junkx")
            raw = arena.f32(1536, "raw")
            hb = [arena.bf16(1024, f"hb{i}") for i in range(2)]
            hT = [arena.bf16(1024, f"hT{i}") for i in range(2)]
            ssx = [arena.f32(8, f"ssx{i}") for i in range(2)]
            ssh = [arena.f32(24, f"ssh{i}") for i in range(2)]
            qkab = [arena.bf16(1024, f"qkab{i}") for i in range(2)]
            qc32 = arena.f32(512, "qc32"); t1 = arena.f32(512, "t1"); t2 = arena.f32(512, "t2")
            qcb = [arena.bf16(512, f"qcb{i}") for i in range(2)]
            qst = [arena.bf16(16 * 128, f"qst{i}") for i in range(2)]
            for q_ in qst:
                fw.op("gpsimd", lambda e, q_=q_: e.memset(q_.ap[:], 0.0), [], [q_.b])
            bT = [banks[0], banks[1]]
            bP = banks[2:6]
            bQ = banks[6]; bR = banks[7]
            bQb = bQ.ap[:, :].bitcast(BF16); bRb = bR.ap[:, :].bitcast(BF16)

            def xload(t):
                x_ = xt[t % 3]
                fw.dma("sync", lambda e, x_=x_, t=t: e.dma_start(out=x_.ap[:], in_=xsrc[t * 128:(t + 1) * 128, :]),
                       [], [x_.b], sem=x_.b)

            def rload(t):
                r_ = rt[t % 2]
                fw.dma("sync", lambda e, r_=r_, t=t: e.dma_start(out=r_.ap[:], in_=rope_d[t]), [], [r_.b], sem=r_.b)

            def stage0a(t):
                i = t % 2
                x_ = xt[t % 3]; sx = ssx[i]
                act(junkx.ap[:], x_.ap[:], AF.Square, [x_.b], [junkx.b, sx.b], accum_out=sx.ap[:, 0:1])
                rsqrt_inplace(sx, sx.ap[:, 0:1], 1.0 / D)
                h_ = hb[i]
                fw.op("vector", lambda e, h_=h_, x_=x_, sx=sx: e.tensor_scalar(out=h_.ap[:], in0=x_.ap[:], scalar1=sx.ap[:, 0:1],
                                                                                scalar2=None, op0=ALU.mult),
                      [x_.b, sx.b], [h_.b])

            def stage0b(t):
                i = t % 2
                h_ = hb[i]
                bt = bT[i]
                btb = bt.ap[:, :].bitcast(BF16)
                for c in range(8):
                    fw.op("tensor", lambda e, c=c, h_=h_, btb=btb: e.transpose(
                        out=btb[:, c * 128:(c + 1) * 128], in_=h_.ap[:, c * 128:(c + 1) * 128], identity=identb.ap[:]),
                        [h_.b, identb.b], [bt.b], pe_acc=True)
                hT_ = hT[i]
                act(hT_.ap[:], btb[:, 0:1024], AF.Copy, [bt.b], [hT_.b])

            def stage1(t):
                i = t % 2
                hT_ = hT[i]
                for g in range(4):
                    for c in range(8):
                        fw.op("tensor", lambda e, g=g, c=c, hT_=hT_: e.matmul(
                            bP[g].ap[:, :], lhsT=hT_.ap[:, c * 128:(c + 1) * 128], rhs=Win.ap[:, c, g * 512:(g + 1) * 512],
                            start=(c == 0), stop=(c == 7)), [hT_.b, Win.b], [bP[g].b], pe_acc=True)
                for g in range(3):
                    act(junk.ap[:, g * 512:(g + 1) * 512], bP[g].ap[:, :], AF.Square, [bP[g].b], [junk.b, sqtok[g]], part=True)
                    fw.op("vector", lambda e, g=g: e.tensor_copy(out=raw.ap[:, g * 512:(g + 1) * 512], in_=bP[g].ap[:, :]),
                          [bP[g].b, sqtok[g]], [raw.b], part=True)
                act(VA.ap[:, t, :, 0:64], bP[3].ap[:, :].rearrange("p (h j) -> p h j", j=64), AF.Copy, [bP[3].b], [VA.b],
                    part=True)

            def stage2(t):
                i = t % 2
                r_ = rt[i]; sh = ssh[i]
                fw.op("vector", lambda e, sh=sh: e.reduce_sum(out=sh.ap[:, 0:24], in_=junk.ap[:].rearrange("p (h j) -> p h j", j=64),
                                                               axis=AX.X), [junk.b], [sh.b])
                rsqrt_inplace(sh, sh.ap[:, 0:24], 1.0 / 64)
                qk_ = qkab[i]
                fw.op("vector", lambda e, qk_=qk_, sh=sh: e.tensor_tensor(
                    out=qk_.ap[:].rearrange("p (h j) -> p h j", j=64),
                    in0=raw.ap[:, 0:1024].rearrange("p (h j) -> p h j", j=64),
                    in1=sh.ap[:, 0:16].unsqueeze(2).to_broadcast([128, 16, 64]), op=ALU.mult),
                    [raw.b, sh.b], [qk_.b])
                fw.op("vector", lambda e, sh=sh: e.tensor_tensor(
                    out=qc32.ap[:].rearrange("p (h j) -> p h j", j=64),
                    in0=raw.ap[:, 1024:1536].rearrange("p (h j) -> p h j", j=64),
                    in1=sh.ap[:, 16:24].unsqueeze(2).to_broadcast([128, 8, 64]), op=ALU.mult),
                    [raw.b, sh.b], [qc32.b])
                fw.op("gpsimd", lambda e: e.tensor_tensor(out=qc32.ap[:], in0=qc32.ap[:], in1=gainC.ap[:], op=ALU.mult),
                      [qc32.b, gainC.b], [qc32.b])
                v5 = lambda ap: ap.rearrange("p (h a w f) -> p h a w f", h=8, a=2, w=2)
                cosb = r_.ap[:, 0:64].unsqueeze(1).to_broadcast([128, 8, 64])
                fw.op("gpsimd", lambda e, cosb=cosb: e.tensor_tensor(out=t1.ap[:].rearrange("p (h j) -> p h j", j=64),
                                                                      in0=qc32.ap[:].rearrange("p (h j) -> p h j", j=64),
                                                                      in1=cosb, op=ALU.mult), [qc32.b, r_.b], [t1.b])
                sin4 = r_.ap[:, 64:128].rearrange("p (a w f) -> p a w f", a=2, w=2)
                q5 = v5(qc32.ap[:]); t25 = v5(t2.ap[:]); t15 = v5(t1.ap[:])
                qcb_ = qcb[i]
                o5 = v5(qcb_.ap[:])
                for w in range(2):
                    sw = sin4[:, :, w, :].unsqueeze(1).to_broadcast([128, 8, 2, 16])
                    fw.op("gpsimd", lambda e, w=w, sw=sw: e.tensor_tensor(out=t25[:, :, :, w, :], in0=q5[:, :, :, 1 - w, :], in1=sw,
                                                                           op=ALU.mult), [qc32.b, r_.b], [t2.b], part=(w > 0))
                fw.op("gpsimd", lambda e, o5=o5: e.tensor_tensor(out=o5[:, :, :, 0, :], in0=t15[:, :, :, 0, :],
                                                                  in1=t25[:, :, :, 0, :], op=ALU.subtract),
                      [t1.b, t2.b], [qcb_.b], part=True)
                fw.op("gpsimd", lambda e, o5=o5: e.tensor_tensor(out=o5[:, :, :, 1, :], in0=t15[:, :, :, 1, :],
                                                                  in1=t25[:, :, :, 1, :], op=ALU.add),
                      [t1.b, t2.b], [qcb_.b], part=True)

            def stage3(t):
                i = t % 2
                qk_ = qkab[i]; qcb_ = qcb[i]
                for b in range(8):
                    fw.op("tensor", lambda e, b=b, qk_=qk_: e.transpose(out=bQb[:, b * 128:(b + 1) * 128],
                                                                        in_=qk_.ap[:, b * 128:(b + 1) * 128],
                                                                        identity=identb.ap[:]),
                          [qk_.b, identb.b], [bQ.b], pe_acc=True)
                for b in range(4):
                    fw.op("tensor", lambda e, b=b, qcb_=qcb_: e.transpose(out=bRb[:, b * 128:(b + 1) * 128],
                                                                          in_=qcb_.ap[:, b * 128:(b + 1) * 128],
                                                                          identity=identb.ap[:]),
                          [qcb_.b, identb.b], [bR.b], pe_acc=True)
                q_ = qst[i]
                q4 = q_.ap[:].rearrange("p (b v t) -> p b v t", b=8, v=2)
                tok = slice(t * 128, (t + 1) * 128)
                for v in range(2):
                    ps = slice(v * 64, (v + 1) * 64)
                    act(q4[ps, 0:3, v, :], bQb[ps, 0:384].rearrange("p (b t) -> p b t", b=3), AF.Copy, [bQ.b, gcol.b], [q_.b],
                        part=True, scale=gcol.ap[ps, 0:1])
                    act(q4[ps, 3:5, v, :], bQb[ps, 512:768].rearrange("p (b t) -> p b t", b=2), AF.Copy, [bQ.b, gcol.b], [q_.b],
                        part=True, scale=gcol.ap[ps, 2:3])
                    fw.op("vector", lambda e, v=v, ps=ps, q4=q4: e.tensor_copy(
                        out=q4[ps, 5:8, v, :], in_=bRb[ps, 0:384].rearrange("p (b t) -> p b t", b=3)), [bR.b], [q_.b], part=True)
                act(KT.ap[:, 0, tok], bQb[:, 384:512], AF.Copy, [bQ.b, gcol.b], [], scale=gcol.ap[:, 1:2])
                act(KT.ap[:, 1:3, tok], bQb[:, 768:1024].rearrange("p (b t) -> p b t", b=2), AF.Copy, [bQ.b, gcol.b], [],
                    scale=gcol.ap[:, 3:4])
                fw.op("vector", lambda e, tok=tok: e.tensor_copy(out=KT.ap[:, 3, tok], in_=bRb[:, 384:512]), [bR.b], [])
                fw.dma("sync", lambda e, q_=q_, tok=tok: e.dma_start(
                    out=qTs[s][:, :, tok].rearrange("b p t -> p b t"), in_=q_.ap[:].rearrange("p (b t) -> p b t", b=16)),
                    [q_.b], [scrbuf], sem=q_.b, part=True)

            xload(0); xload(1); xload(2)
            rload(0); rload(1)
            stage0a(0); xload(3); stage0a(1); xload(4)
            stage0b(0); stage0a(2); stage0b(1); stage1(0); stage0a(3); stage2(0)
            for t in range(NT):
                if t + 2 < NT:
                    stage0b(t + 2)
                if t + 1 < NT:
                    stage1(t + 1)
                    stage2(t + 1)
                stage3(t)
                if t + 5 < NT:
                    xload(t + 5)
                if t + 4 < NT:
                    stage0a(t + 4)
                if t + 2 < NT:
                    rload(t + 2)
            fw.barrier()
            fw.scope_end(sc0)
            arena.release(m0)

        def phase_att(s, l, KT, VA, xsrc, Wout):
            m0 = arena.mark()
            sc0 = fw.scope_begin()
            QT = [arena.bf16(16 * 512, f"QT{i}") for i in range(2)]
            for q in QT:
                q.ap = q.ap.rearrange("p (b t) -> p b t", b=16)
            NPT = 3
            PT = [arena.bf16(1536, f"PT{i}") for i in range(NPT)]
            oall = arena.f32(4 * 1024, "oall")
            oall4 = oall.ap[:].rearrange("p (t h j) -> p t h j", t=4, h=16)
            onb = [arena.bf16(1024, f"onb{i}") for i in range(2)]
            onT = [arena.bf16(1024, f"onT{i}") for i in range(2)]
            xt = [arena.f32(1024, f"axt{i}") for i in range(4)]
            junk = arena.f32(1024, "ajunk")
            ss3 = [arena.f32(8, f"ss3{i}") for i in range(2)]
            rden = arena.f32(16, "rden")
            sslots = [(banks[0:3], fw.buf("ss0")), (banks[3:6], fw.buf("ss1"))]
            ob = [banks[6], banks[7]]
            state = {"s": 0, "p": 0}
            from collections import deque
            fillers = deque()

            def next_s():
                b = sslots[state["s"] % 2]
                state["s"] += 1
                return b

            def next_p():
                b = PT[state["p"] % NPT]
                state["p"] += 1
                return b

            def slot_ap(slot, a, b):
                return psall[:, slot[0][0].col0 + a:slot[0][0].col0 + b]

            def run_units(units, L=2):
                n = len(units)
                pts = [None] * n
                for k in range(n + L):
                    if k < n:
                        slot = next_s()
                        pt = next_p()
                        ncols = units[k]["qk"](slot)
                        act(pt.ap[:, 0:ncols], slot_ap(slot, 0, ncols), AF.Exp, [slot[1]], [pt.b], scale=0.125)
                        pts[k] = pt
                    if k - L >= 0:
                        units[k - L]["pv"](pts[k - L])
                    if fillers:
                        fillers.popleft()()

            def mm(out, lhsT, rhs, start, stop, reads, wbuf):
                fw.op("tensor", lambda e: e.matmul(out, lhsT=lhsT, rhs=rhs, start=start, stop=stop,
                                                    skip_group_check=True), reads, [wbuf], pe_acc=True)

            def q_load(qc):
                Q = QT[qc % 2]
                fw.dma("sync", lambda e, Q=Q, qc=qc: e.dma_start(
                    out=Q.ap[:], in_=qTs[s][:, :, qc * 512:(qc + 1) * 512].rearrange("b p t -> p b t")),
                    [], [Q.b], sem=Q.b)

            def x_loads(qc):
                for qt in range(4):
                    t = qc * 4 + qt
                    x_ = xt[qt]
                    fw.dma("sync", lambda e, x_=x_, t=t: e.dma_start(out=x_.ap[:], in_=xsrc[t * 128:(t + 1) * 128, :]),
                           [], [x_.b], sem=x_.b)

            def evacuate(kind, idx):
                for bi in range(2):
                    bk = ob[bi]
                    if kind == "B":
                        qt0 = idx * 2 + bi
                        src = bk.ap[:, 0:260].rearrange("p (g c) -> p g c", c=65)
                        dst = oall4[:, qt0, 6:10, :]
                        rd = rden.ap[:, 0:4]
                        fw.op("vector", lambda e, src=src, rd=rd: e.reciprocal(out=rd, in_=src[:, :, 64]),
                              [bk.b], [rden.b])
                        fw.op("vector", lambda e, src=src, dst=dst, rd=rd: e.tensor_tensor(
                            out=dst, in0=src[:, :, 0:64], in1=rd.unsqueeze(2).to_broadcast([128, 4, 64]), op=ALU.mult),
                            [bk.b, rden.b], [oall.b], part=True)
                    else:
                        hbase = (0 if kind == "A" else 10) + 3 * idx
                        src = bk.ap[:, 0:390].rearrange("p (g c) -> p g c", c=65)
                        rd = rden.ap[:, 0:6]
                        if kind == "A":
                            es = esA.ap[:, 3 * idx:3 * idx + 3].unsqueeze(1).to_broadcast([128, 2, 3])
                            fw.op("vector", lambda e, src=src, rd=rd, es=es: e.tensor_tensor(
                                out=rd.rearrange("p (a b) -> p a b", a=2),
                                in0=src[:, :, 64].rearrange("p (a b) -> p a b", a=2), in1=es, op=ALU.add),
                                [bk.b, esA.b], [rden.b])
                            fw.op("vector", lambda e, rd=rd: e.reciprocal(out=rd, in_=rd), [rden.b], [rden.b])
                        else:
                            fw.op("vector", lambda e, src=src, rd=rd: e.reciprocal(out=rd, in_=src[:, :, 64]),
                                  [bk.b], [rden.b])
                        dst = oall4[:, bi * 2:bi * 2 + 2, hbase:hbase + 3, :]
                        fw.op("vector", lambda e, src=src, dst=dst, rd=rd: e.tensor_tensor(
                            out=dst, in0=src[:, :, 0:64].rearrange("p (a b) c -> p a b c", a=2),
                            in1=rd.rearrange("p (a b) -> p a b", a=2).unsqueeze(3).to_broadcast([128, 2, 3, 64]), op=ALU.mult),
                            [bk.b, rden.b], [oall.b], part=True)

            def finalize_steps(qc):
                steps = []
                for qt in range(4):
                    t = qc * 4 + qt
                    i2 = t % 2
                    x_ = xt[qt]
                    o_ = oall.ap[:, qt * 1024:(qt + 1) * 1024]
                    segs = [(0, 384), (384, 640), (640, 1024)]
                    s3 = ss3[i2]
                    on_ = onb[i2]
                    oT_ = onT[i2]

                    def st_a(o_=o_, s3=s3):
                        for gi, (a, b) in enumerate(segs):
                            act(junk.ap[:, a:b], o_[:, a:b], AF.Square, [oall.b], [junk.b, s3.b], part=(gi > 0),
                                accum_out=s3.ap[:, gi:gi + 1])

                    def st_b(s3=s3):
                        fw.op("vector", lambda e: e.tensor_tensor(out=s3.ap[:, 0:3], in0=s3.ap[:, 0:3], in1=invn3.ap[:],
                                                                   op=ALU.mult), [s3.b, invn3.b], [s3.b])
                        rsqrt_inplace(s3, s3.ap[:, 0:3], 1.0)

                    def st_c(o_=o_, s3=s3, on_=on_):
                        for gi, (a, b) in enumerate(segs):
                            fw.op("vector", lambda e, a=a, b=b, gi=gi: e.tensor_scalar(
                                out=on_.ap[:, a:b], in0=o_[:, a:b], scalar1=s3.ap[:, gi:gi + 1], scalar2=None, op0=ALU.mult),
                                [oall.b, s3.b], [on_.b], part=(gi > 0))

                    box = {}

                    def st_d(on_=on_, oT_=oT_, box=box):
                        slot = next_s()
                        box["slot"] = slot
                        btb = slot[0][0].ap[:, :].bitcast(BF16)
                        for c in range(8):
                            fw.op("tensor", lambda e, c=c: e.transpose(
                                out=btb[:, c * 128:(c + 1) * 128], in_=on_.ap[:, c * 128:(c + 1) * 128], identity=identb.ap[:]),
                                [on_.b, identb.b], [slot[1]], pe_acc=True)
                        fw.op("vector", lambda e: e.tensor_copy(out=oT_.ap[:], in_=btb[:, 0:1024]), [slot[1]], [oT_.b])

                    def st_e(hf, oT_=oT_, x_=x_, t=t):
                        slot = next_s()
                        by = slot[0][0]
                        for c in range(8):
                            fw.op("tensor", lambda e, c=c: e.matmul(
                                by.ap[:, :], lhsT=oT_.ap[:, c * 128:(c + 1) * 128], rhs=Wout.ap[:, c, hf * 512:(hf + 1) * 512],
                                start=(c == 0), stop=(c == 7)), [oT_.b, Wout.b], [slot[1]], pe_acc=True)
                        fw.op("vector", lambda e: e.tensor_tensor(
                            out=x_.ap[:, hf * 512:(hf + 1) * 512], in0=by.ap[:, :], in1=x_.ap[:, hf * 512:(hf + 1) * 512],
                            op=ALU.add), [slot[1], x_.b], [x_.b])
                        if hf == 1:
                            fw.dma("sync", lambda e: e.dma_start(out=x2s[s][t * 128:(t + 1) * 128, :], in_=x_.ap[:]),
                                   [x_.b], [scrbuf], sem=x_.b, part=True)

                    st_e0 = lambda st_e=st_e: st_e(0)
                    st_e1 = lambda st_e=st_e: st_e(1)
                    steps.append([st_a, st_b, st_c, st_d, st_e0, st_e1])
                out = []
                for pr_ in range(2):
                    for k in range(6):
                        out += [steps[2 * pr_][k], steps[2 * pr_ + 1][k]]
                        if k < 4:
                            out.append(lambda: None)
                return out

            q_load(0)
            x_loads(0)
            for qc in range(8):
                Q = QT[qc % 2]
                Q4 = Q.ap[:].rearrange("p (b v) t -> p b v t", v=2)
                if qc + 1 < 8:
                    q_load(qc + 1)
                parts = []
                for j in range(2):
                    started = set()
                    units = []
                    for kb in range(NT):
                        def qk(slot, kb=kb, j=j, Q4=Q4):
                            for hh in range(3):
                                bk = slot[0][hh]
                                mm(bk.ap[:, :], KT.ap[:, 3, kb * 128:(kb + 1) * 128], Q4[:, 5 + hh, j, :],
                                   True, True, [Q.b], slot[1])
                            return 1536

                        def pv(pt, kb=kb, j=j, started=started):
                            for hh in range(3):
                                for qt in range(4):
                                    g = qt * 3 + hh
                                    bk = ob[g // 6]
                                    col = (g % 6) * 65
                                    first = bk not in started
                                    started.add(bk)
                                    mm(bk.ap[:, col:col + 65], pt.ap[:, hh * 512 + qt * 128:hh * 512 + (qt + 1) * 128],
                                       VA.ap[:, kb, 6 + j, :], first, kb == NT - 1, [pt.b], bk.b)
                        units.append({"qk": qk, "pv": pv})
                    parts.append(("C", j, units))
                for j in range(2):
                    started = set()
                    units = []
                    for qt in range(4):
                        i = qc * 4 + qt
                        for o in (-1, 0, 1):
                            kb = i + o
                            if kb < 0 or kb >= NT:
                                continue

                            def qk(slot, kb=kb, qt=qt, o=o, j=j, Q4=Q4):
                                bk = slot[0][0]
                                mm(bk.ap[:, 0:384].rearrange("p (h t) -> p h t", h=3),
                                   KT.ap[:, 0, kb * 128:(kb + 1) * 128], Q4[:, 0:3, j, qt * 128:(qt + 1) * 128],
                                   True, False, [Q.b], slot[1])
                                mm(bk.ap[:, 0:384], identb.ap[:], abias.ap[:, 0, (o + 1) * 2 + j, :], False, False,
                                   [abias.b, identb.b], slot[1])
                                mm(bk.ap[:, 0:384], identb.ap[:], abias.ap[:, 1, (o + 1) * 2 + j, :], False, True,
                                   [abias.b, identb.b], slot[1])
                                return 384

                            def pv(pt, kb=kb, qt=qt, j=j, started=started, last=(o == 1 or kb == NT - 1)):
                                for hh in range(3):
                                    g = qt * 3 + hh
                                    bk = ob[g // 6]
                                    col = (g % 6) * 65
                                    first = bk not in started
                                    started.add(bk)
                                    mm(bk.ap[:, col:col + 65], pt.ap[:, hh * 128:(hh + 1) * 128], VA.ap[:, kb, 0 + j, :],
                                       first, last, [pt.b], bk.b)
                            units.append({"qk": qk, "pv": pv})
                    parts.append(("A", j, units))
                for qp in range(2):
                    started = set()
                    units = []
                    for ql in range(2):
                        qt = qp * 2 + ql
                        i = qc * 4 + qt
                        js = _b_rows(i)
                        for kb in js:
                            def qk(slot, kb=kb, qt=qt, i=i, Q4=Q4):
                                bk = slot[0][0]
                                for hbq in range(4):
                                    mm(bk.ap[:, hbq * 128:(hbq + 1) * 128], KT.ap[:, 1 + hbq // 2, kb * 128:(kb + 1) * 128],
                                       Q4[:, 3 + hbq // 2, hbq % 2, qt * 128:(qt + 1) * 128], hbq == 0, False, [Q.b], slot[1])
                                oi = kb - i + 3
                                mm(bk.ap[:, 0:512], identb.ap[:], rbt.ap[:, 0, oi, :], False, False, [identb.b], slot[1])
                                mm(bk.ap[:, 0:512], identb.ap[:], rbt.ap[:, 1, oi, :], False, False, [identb.b], slot[1])
                                mk = maskb.ap[:, pidx[(i, kb)], :]
                                for hbq in range(4):
                                    mm(bk.ap[:, hbq * 128:(hbq + 1) * 128], identb.ap[:], mk, False, hbq == 3, [maskb.b], slot[1])
                                return 512

                            def pv(pt, kb=kb, ql=ql, started=started, last=(kb == js[-1])):
                                for hbq in range(4):
                                    bk = ob[ql]
                                    col = hbq * 65
                                    first = bk not in started
                                    started.add(bk)
                                    mm(bk.ap[:, col:col + 65], pt.ap[:, hbq * 128:(hbq + 1) * 128], VA.ap[:, kb, 2 + hbq, :],
                                       first, last, [pt.b], bk.b)
                            units.append({"qk": qk, "pv": pv})
                    parts.append(("B", qp, units))

                for pi, (kind, idx, units) in enumerate(parts):
                    run_units(units)
                    evacuate(kind, idx)
                while fillers:
                    fillers.popleft()()
                fillers.extend(finalize_steps(qc))
                if qc + 1 < 8:
                    fillers.append(lambda qc=qc: x_loads(qc + 1))
            while fillers:
                fillers.popleft()()
            fw.barrier()
            fw.scope_end(sc0)
            arena.release(m0)

        def phase_moe(s, l, dst):
            from collections import deque
            m0 = arena.mark()
            sc0 = fw.scope_begin()
            ST = 1024
            NSUB = ST // 128
            NS = NSUB
            nst = T // ST
            yacc = arena.f32(NSUB * 1024, "yacc")
            yacc3 = yacc.ap[:].rearrange("p (s d) -> p s d", s=NSUB)
            h2Ts = [arena.bf16(8 * ST, f"h2T{i}") for i in range(2)]
            for h in h2Ts:
                h.ap = h.ap.rearrange("p (c t) -> p c t", c=8)
            gatess = [arena.f32(NSUB * 16, f"gates{i}") for i in range(2)]
            h2f = [arena.f32(1024, f"h2f{i}") for i in range(2)]
            hTf = [arena.f32(1024, f"hTf{i}") for i in range(2)]
            xe = [arena.f32(1024, f"xe{i}") for i in range(4)]
            junk = arena.f32(1024, "mjunk")
            ssx = [arena.f32(8, f"mssx{i}") for i in range(2)]
            lg = arena.f32(NSUB * 20, "lg")
            gt = {n: arena.f32(NSUB * 16, "g_" + n) for n in ("a",)}
            gs = {n: arena.f32(NSUB * 4, "s_" + n) for n in ("gmax", "pg", "ohg", "ex", "esel", "oh1", "es2", "oh2",
                                                             "m1", "m2", "e2", "w1", "w2", "ws")}
            Wg = [arena.bf16(8 * FE, f"Wg{i}") for i in range(2)]
            Wu = [arena.bf16(8 * FE, f"Wu{i}") for i in range(2)]
            Wd = [arena.bf16(4 * D, f"Wd{i}") for i in range(2)]
            for w in Wg + Wu:
                w.ap = w.ap.rearrange("p (c f) -> p c f", c=8)
            for w in Wd:
                w.ap = w.ap.rearrange("p (c d) -> p c d", c=4)
            aT = [arena.bf16(4 * 512, f"aT{i}") for i in range(2)]
            for a in aT:
                a.ap = a.ap.rearrange("p (c t) -> p c t", c=4)
            sg = [arena.f32(512, f"sg{i}") for i in range(2)]
            bGU = [(banks[0], banks[1]), (banks[2], banks[3])]
            bY = [banks[4], banks[5], banks[6], banks[7]]
            cnt = {"gu": 0, "y": 0, "sg": 0}

            def next_y():
                b = bY[cnt["y"] % 4]
                cnt["y"] += 1
                return b

            def load_expert(e_, slot):
                fw.dma("gpsimd", lambda e: e.dma_start(out=Wg[slot].ap[:], in_=wg_d[l, e_].rearrange("(c p) f -> p c f", p=128)),
                       [], [Wg[slot].b], sem=Wg[slot].b)
                fw.dma("gpsimd", lambda e: e.dma_start(out=Wu[slot].ap[:], in_=wu_d[l, e_].rearrange("(c p) f -> p c f", p=128)),
                       [], [Wu[slot].b], sem=Wu[slot].b)
                fw.dma("gpsimd", lambda e: e.dma_start(out=Wd[slot].ap[:], in_=wd_d[l, e_].rearrange("(c p) d -> p c d", p=128)),
                       [], [Wd[slot].b], sem=Wd[slot].b)

            lg3 = lg.ap[:].rearrange("p (s n) -> p s n", s=NS)

            def prologue_steps(sti):
                kb = sti % 2
                h2T = h2Ts[kb]
                gates = gatess[kb]

                def st_i(sub):
                    t = sti * NSUB + sub
                    hf_ = h2f[sub % 2]; sx = ssx[sub % 2]
                    fw.dma("sync", lambda e: e.dma_start(out=hf_.ap[:], in_=x2s[s][t * 128:(t + 1) * 128, :]),
                           [], [hf_.b], sem=hf_.b)
                    act(junk.ap[:], hf_.ap[:], AF.Square, [hf_.b], [junk.b, sx.b], accum_out=sx.ap[:, 0:1])
                    rsqrt_inplace(sx, sx.ap[:, 0:1], 1.0 / D)
                    fw.op("vector", lambda e: e.tensor_scalar(out=hf_.ap[:], in0=hf_.ap[:], scalar1=sx.ap[:, 0:1],
                                                               scalar2=None, op0=ALU.mult), [hf_.b, sx.b], [hf_.b])

                def st_ii(sub):
                    hf_ = h2f[sub % 2]; hT_ = hTf[sub % 2]
                    for hh in range(2):
                        bt = next_y()
                        for c in range(4):
                            cc = hh * 4 + c
                            fw.op("tensor", lambda e, c=c, cc=cc, bt=bt: e.transpose(
                                out=bt.ap[:, c * 128:(c + 1) * 128], in_=hf_.ap[:, cc * 128:(cc + 1) * 128], identity=identf.ap[:]),
                                [hf_.b, identf.b], [bt.b], pe_acc=True)
                        fw.op("vector", lambda e, hh=hh, bt=bt: e.tensor_tensor(
                            out=hT_.ap[:, hh * 512:(hh + 1) * 512].rearrange("p (c t) -> p c t", c=4),
                            in0=bt.ap[:, :].rearrange("p (c t) -> p c t", c=4),
                            in1=g2.ap[:, hh * 4:(hh + 1) * 4].unsqueeze(2).to_broadcast([128, 4, 128]), op=ALU.mult),
                            [bt.b, g2.b], [hT_.b], part=(hh > 0))
                    act(h2T.ap[:, :, sub * 128:(sub + 1) * 128], hT_.ap[:].rearrange("p (c t) -> p c t", c=8), AF.Copy,
                        [hT_.b], [h2T.b], part=True)

                def st_iii(sub):
                    hT_ = hTf[sub % 2]
                    bt = next_y()
                    for c in range(8):
                        fw.op("tensor", lambda e, c=c: e.matmul(
                            bt.ap[:, 0:20], lhsT=hT_.ap[:, c * 128:(c + 1) * 128], rhs=wr.ap[:, c, :],
                            start=(c == 0), stop=(c == 7)), [hT_.b, wr.b], [bt.b], pe_acc=True)
                    fw.op("vector", lambda e: e.tensor_tensor(out=lg3[:, sub, :], in0=bt.ap[:, 0:20], in1=rb.ap[:], op=ALU.add),
                          [bt.b, rb.b], [lg.b], part=True)

                def st_gate():
                    gl = lg3[:, :, 0:4]
                    el = lg3[:, :, 4:20].rearrange("p s (g e) -> p s g e", g=4)

                    def S3(n):
                        return gs[n].ap[:].rearrange("p (s e) -> p s e", s=NS)

                    def S1(n):
                        return gs[n].ap[:, 0:NS]

                    def vop(fn, reads, writes):
                        fw.op("vector", fn, reads, writes)

                    bc4 = lambda ap: ap.unsqueeze(2).to_broadcast([128, NS, 4])
                    vop(lambda e: e.tensor_reduce(out=S1("gmax"), in_=gl, axis=AX.X, op=ALU.max), [lg.b], [gs["gmax"].b])
                    vop(lambda e: e.tensor_tensor(out=S3("ohg"), in0=gl, in1=bc4(S1("gmax")), op=ALU.is_equal),
                        [lg.b, gs["gmax"].b], [gs["ohg"].b])
                    vop(lambda e: e.tensor_tensor(out=S3("ex"), in0=gl, in1=bc4(S1("gmax")), op=ALU.subtract),
                        [lg.b, gs["gmax"].b], [gs["ex"].b])
                    act(S3("ex"), S3("ex"), AF.Exp, [gs["ex"].b], [gs["ex"].b])
                    vop(lambda e: e.reduce_sum(out=S1("pg"), in_=S3("ex"), axis=AX.X), [gs["ex"].b], [gs["pg"].b])
                    vop(lambda e: e.reciprocal(out=S1("pg"), in_=S1("pg")), [gs["pg"].b], [gs["pg"].b])
                    ga4 = gt["a"].ap[:].rearrange("p (s g e) -> p s g e", s=NS, g=4)
                    vop(lambda e: e.tensor_tensor(out=ga4, in0=el, in1=S3("ohg").unsqueeze(3).to_broadcast([128, NS, 4, 4]),
                                                  op=ALU.mult), [lg.b, gs["ohg"].b], [gt["a"].b])
                    vop(lambda e: e.reduce_sum(out=S3("esel"), in_=gt["a"].ap[:].rearrange("p (s g e) -> p s e g", s=NS, g=4),
                                               axis=AX.X), [gt["a"].b], [gs["esel"].b])
                    vop(lambda e: e.tensor_reduce(out=S1("m1"), in_=S3("esel"), axis=AX.X, op=ALU.max), [gs["esel"].b], [gs["m1"].b])
                    vop(lambda e: e.tensor_tensor(out=S3("oh1"), in0=S3("esel"), in1=bc4(S1("m1")), op=ALU.is_equal),
                        [gs["esel"].b, gs["m1"].b], [gs["oh1"].b])
                    vop(lambda e: e.scalar_tensor_tensor(out=S3("es2"), in0=S3("oh1"), scalar=NEG, in1=S3("esel"),
                                                         op0=ALU.mult, op1=ALU.add), [gs["oh1"].b, gs["esel"].b], [gs["es2"].b])
                    vop(lambda e: e.tensor_reduce(out=S1("m2"), in_=S3("es2"), axis=AX.X, op=ALU.max), [gs["es2"].b], [gs["m2"].b])
                    vop(lambda e: e.tensor_tensor(out=S3("oh2"), in0=S3("es2"), in1=bc4(S1("m2")), op=ALU.is_equal),
                        [gs["es2"].b, gs["m2"].b], [gs["oh2"].b])
                    vop(lambda e: e.tensor_tensor(out=S1("e2"), in0=S1("m2"), in1=S1("m1"), op=ALU.subtract),
                        [gs["m1"].b, gs["m2"].b], [gs["e2"].b])
                    act(S1("e2"), S1("e2"), AF.Exp, [gs["e2"].b], [gs["e2"].b])
                    vop(lambda e: e.tensor_scalar(out=S1("w1"), in0=S1("e2"), scalar1=1.0, scalar2=None, op0=ALU.add),
                        [gs["e2"].b], [gs["w1"].b])
                    vop(lambda e: e.reciprocal(out=S1("w1"), in_=S1("w1")), [gs["w1"].b], [gs["w1"].b])
                    vop(lambda e: e.tensor_tensor(out=S1("w1"), in0=S1("w1"), in1=S1("pg"), op=ALU.mult),
                        [gs["w1"].b, gs["pg"].b], [gs["w1"].b])
                    vop(lambda e: e.tensor_tensor(out=S1("w2"), in0=S1("w1"), in1=S1("e2"), op=ALU.mult),
                        [gs["w1"].b, gs["e2"].b], [gs["w2"].b])
                    vop(lambda e: e.tensor_tensor(out=S3("ws"), in0=S3("oh1"), in1=bc4(S1("w1")), op=ALU.mult),
                        [gs["oh1"].b, gs["w1"].b], [gs["ws"].b])
                    vop(lambda e: e.tensor_tensor(out=S3("oh2"), in0=S3("oh2"), in1=bc4(S1("w2")), op=ALU.mult),
                        [gs["oh2"].b, gs["w2"].b], [gs["oh2"].b])
                    vop(lambda e: e.tensor_tensor(out=S3("ws"), in0=S3("ws"), in1=S3("oh2"), op=ALU.add),
                        [gs["ws"].b, gs["oh2"].b], [gs["ws"].b])
                    g4 = gates.ap[:].rearrange("p (s g e) -> p s g e", s=NS, g=4)
                    vop(lambda e: e.tensor_tensor(out=g4, in0=S3("ohg").unsqueeze(3).to_broadcast([128, NS, 4, 4]),
                                                  in1=S3("ws").unsqueeze(2).to_broadcast([128, NS, 4, 4]), op=ALU.mult),
                        [gs["ohg"].b, gs["ws"].b], [gates.b])

                steps = []
                for j in range(NSUB + 2):
                    def pos(j=j):
                        if j < NSUB:
                            st_i(j)
                        if 0 <= j - 1 < NSUB:
                            st_ii(j - 1)
                        if 0 <= j - 2 < NSUB:
                            st_iii(j - 2)
                    steps.append(pos)
                steps.append(st_gate)
                return steps

            xpool = xe + h2f + hTf

            def epi_load(sti, sub):
                t = sti * NSUB + sub
                x_ = xpool[sub]
                fw.dma("sync", lambda e: e.dma_start(out=x_.ap[:], in_=x2s[s][t * 128:(t + 1) * 128, :]), [], [x_.b], sem=x_.b)

            def epilogue(sti):
                for sub in range(NSUB):
                    t = sti * NSUB + sub
                    x_ = xpool[sub]
                    fw.op("vector", lambda e, x_=x_, sub=sub: e.tensor_tensor(out=x_.ap[:], in0=x_.ap[:], in1=yacc3[:, sub, :],
                                                                                op=ALU.add), [x_.b, yacc.b], [x_.b])
                    fw.dma("sync", lambda e, x_=x_, t=t: e.dma_start(out=dst[t * 128:(t + 1) * 128, :], in_=x_.ap[:]),
                           [x_.b], [outbuf], sem=x_.b, part=True)

            def GU(sti, e_, tt, slot):
                h2T = h2Ts[sti % 2]
                wg_, wu_ = Wg[slot], Wu[slot]
                a_ = aT[tt % 2]
                for fc in range(4):
                    bg, bu = bGU[cnt["gu"] % 2]
                    cnt["gu"] += 1
                    for (bk, w_) in ((bg, wg_), (bu, wu_)):
                        for c in range(8):
                            fw.op("tensor", lambda e, bk=bk, w_=w_, c=c, fc=fc: e.matmul(
                                bk.ap[:, :], lhsT=w_.ap[:, c, fc * 128:(fc + 1) * 128],
                                rhs=h2T.ap[:, c, tt * 512:(tt + 1) * 512], start=(c == 0), stop=(c == 7)),
                                [w_.b, h2T.b], [bk.b], pe_acc=True)
                    sg_ = sg[cnt["sg"] % 2]
                    cnt["sg"] += 1
                    act(sg_.ap[:], bg.ap[:, :], AF.Silu, [bg.b], [sg_.b])
                    fw.op("vector", lambda e, fc=fc, sg_=sg_, bu=bu: e.tensor_tensor(
                        out=a_.ap[:, fc, :], in0=bu.ap[:, :], in1=sg_.ap[:], op=ALU.mult),
                        [bu.b, sg_.b], [a_.b], part=(fc > 0))

            def DOWN(sti, e_, tt, slot):
                gates3 = gatess[sti % 2].ap[:].rearrange("p (s n) -> p s n", s=NS)
                wd_ = Wd[slot]
                a_ = aT[tt % 2]
                for sl in range(4):
                    sub = tt * 4 + sl
                    for hf in range(2):
                        by = next_y()
                        for fc in range(4):
                            fw.op("tensor", lambda e, by=by, fc=fc, sl=sl, hf=hf: e.matmul(
                                by.ap[:, :], lhsT=a_.ap[:, fc, sl * 128:(sl + 1) * 128],
                                rhs=wd_.ap[:, fc, hf * 512:(hf + 1) * 512], start=(fc == 0), stop=(fc == 3)),
                                [a_.b, wd_.b], [by.b], pe_acc=True)
                        ya = yacc3[:, sub, hf * 512:(hf + 1) * 512]
                        gsc = gates3[:, sub, e_:e_ + 1]
                        if e_ == 0:
                            fw.op("vector", lambda e, by=by, ya=ya, gsc=gsc: e.tensor_scalar(
                                out=ya, in0=by.ap[:, :], scalar1=gsc, scalar2=None, op0=ALU.mult),
                                [by.b, gatess[sti % 2].b], [yacc.b], part=True)
                        else:
                            fw.op("vector", lambda e, by=by, ya=ya, gsc=gsc: e.scalar_tensor_tensor(
                                out=ya, in0=by.ap[:, :], scalar=gsc, in1=ya, op0=ALU.mult, op1=ALU.add),
                                [by.b, gatess[sti % 2].b, yacc.b], [yacc.b], part=True)

            tiles = [(sti, e_, tt) for sti in range(nst) for e_ in range(NE) for tt in range(ST // 512)]
            fq = deque()
            load_expert(0, 0)
            for st_ in prologue_steps(0):
                st_()
            nload = 1
            slot_of = {}
            prev = None
            for n, (sti, e_, tt) in enumerate(tiles):
                if tt == 0:
                    slot_of[(sti, e_)] = (nload - 1) % 2
                    if e_ == 2 and sti + 1 < nst:
                        fq.extend(prologue_steps(sti + 1))
                    if e_ == NE - 1:
                        for sub in range(NSUB):
                            epi_load(sti, sub)
                slot = slot_of[(sti, e_)]
                GU(sti, e_, tt, slot)
                if prev is not None:
                    DOWN(*prev)
                    if prev[1] == NE - 1 and prev[2] == ST // 512 - 1:
                        epilogue(prev[0])
                if tt == 0:
                    if n + 2 < len(tiles):
                        load_expert((e_ + 1) % NE, nload % 2)
                    nload += 1
                prev = (sti, e_, tt, slot)
                if fq:
                    fq.popleft()()
            DOWN(*prev)
            epilogue(prev[0])
            fw.barrier()
            fw.scope_end(sc0)
            arena.release(m0)

        done = False
        for l in range(NL):
            if done:
                break
            layer_setup(l)
            for s in range(NSEQ):
                xsrc = xin[s] if l == 0 else xl[s]
                ms = arena.mark()
                scs = fw.scope_begin()
                Wout = arena.bf16(8 * 1024, "Wout"); Wout.ap = Wout.ap.rearrange("p (c n) -> p c n", c=8)
                KT = arena.bf16(4 * T, "KT"); KT.ap = KT.ap.rearrange("p (b t) -> p b t", b=4)
                VA = arena.bf16(NT * 8 * 66, "VA"); VA.ap = VA.ap.rearrange("p (t h c) -> p t h c", t=NT, h=8)
                VA.ap = VA.ap[:, :, :, 0:65]
                def wout_hook(l=l, Wout=Wout):
                    fw.dma("gpsimd", lambda e: e.dma_start(
                        out=Wout.ap[:], in_=w_out_d[l].rearrange("(c p) n -> p c n", p=128)), [], [Wout.b], sem=Wout.b)
                    for c in range(8):
                        fw.op("vector", lambda e, c=c: e.tensor_scalar(
                            out=Wout.ap[:, c, :], in0=Wout.ap[:, c, :], scalar1=og.ap[:, c:c + 1], scalar2=None, op0=ALU.mult),
                            [Wout.b, og.b], [Wout.b])
                fw.op("gpsimd", lambda e, VA=VA: e.memset(VA.ap[:, :, :, 64:65], 1.0), [], [VA.b])
                phase_p1(s, l, KT, VA, xsrc, wout_hook)
                if stop_after == ("p1", l, s):
                    done = True
                    break
                phase_att(s, l, KT, VA, xsrc, Wout)
                fw.scope_end(scs)
                arena.release(ms)
                if stop_after == ("att", l, s):
                    done = True
                    break
                dst = yout[s] if l == NL - 1 else xl[s]
                phase_moe(s, l, dst)
        fw.barrier()
        fw.emit()
    return nc


_NC_CACHE = {}


def _prep_shared(ln1, w_in, qk_gain, sink, rpb, out_gain, w_out, ln2, w_router_group, b_router_group,
                 w_router_expert, b_router_expert, w_gate, w_up, w_down):
    C = _get_consts()
    f = lambda a: np.ascontiguousarray(np.asarray(a, dtype=np.float32))
    w_in_f = f(w_in)
    perm = []
    for base in (0, 1408):
        blk = []
        for b_ in range(3):
            blk += list(range(base + b_ * 64, base + b_ * 64 + 64)) + list(range(base + (b_ + 3) * 64, base + (b_ + 3) * 64 + 64))
        perm.append(blk)
    cols = (perm[0] + list(range(384, 512)) + list(range(640, 1152)) + perm[1] + list(range(1792, 1920))
            + list(range(512, 640)) + list(range(1152, 1408)) + list(range(1920, 2048)))
    assert len(cols) == 2048 and len(set(cols)) == 2048
    w_in_p = np.ascontiguousarray(w_in_f[:, :, cols])
    pc = lambda a: np.ascontiguousarray(f(a).reshape(DEPTH, 8, 128).transpose(0, 2, 1))
    w_r = np.concatenate([f(w_router_group), f(w_router_expert)], axis=2)
    w_r = np.ascontiguousarray(w_r.reshape(DEPTH, 8, 128, 20).transpose(0, 2, 1, 3))
    qg = f(qk_gain)
    gcolh = np.stack([np.concatenate([qg[:, m, r, :], qg[:, m, r, :]], axis=1) for m in range(2) for r in range(2)], axis=2)
    shared = {
        "ln1": pc(ln1), "w_in": w_in_p, "qk_gain": qg, "sink": f(sink), "rpb": f(rpb),
        "out_gain": pc(out_gain), "w_out": f(w_out), "ln2": pc(ln2),
        "w_router": w_r, "gcolh": np.ascontiguousarray(gcolh), "b_router_group": f(b_router_group),
        "b_router_expert": f(b_router_expert),
        "w_gate": f(w_gate), "w_up": f(w_up), "w_down": f(w_down),
        "c_rope": C["rope"], "c_abias": C["abias"], "c_maskb": C["maskb"], "c_jperm": C["jperm"],
        "c_invn3": C["invn3"],
    }
    return shared


def kernel(x_prompt, x_sample, ln1, w_in, qk_gain, sink, rpb, out_gain, w_out, ln2,
           w_router_group, b_router_group, w_router_expert, b_router_expert, w_gate, w_up, w_down):
    C = _get_consts()
    f = lambda a: np.ascontiguousarray(np.asarray(a, dtype=np.float32))
    seqs = [f(x_prompt[i]) for i in range(x_prompt.shape[0])] + [f(x_sample[i]) for i in range(x_sample.shape[0])]
    nseq = len(seqs)
    slot0 = list(range(8))
    slot1 = {0: 8, 1: 9, 4: 10, 5: 11}
    zeros = np.zeros((T, D), np.float32)
    if "nc" not in _NC_CACHE:
        _NC_CACHE["nc"] = build()
    nc = _NC_CACHE["nc"]
    shared = _prep_shared(ln1, w_in, qk_gain, sink, rpb, out_gain, w_out, ln2, w_router_group, b_router_group,
                          w_router_expert, b_router_expert, w_gate, w_up, w_down)
    in_maps = []
    for c in range(NCORES):
        m = dict(shared)
        m["x0"] = seqs[slot0[c]]
        m["x1"] = seqs[slot1[c]] if c in slot1 else zeros
        in_maps.append(m)
    res = run_bass_kernel_spmd(nc, in_maps, core_ids=list(range(NCORES)))
    outs = [None] * nseq
    for c in range(NCORES):
        outs[slot0[c]] = res.results[c]["y0"]
        if c in slot1:
            outs[slot1[c]] = res.results[c]["y1"]
    nb = x_prompt.shape[0]
    y_prompt = np.stack(outs[:nb], axis=0).astype(np.float32)
    y_sample = np.stack(outs[nb:], axis=0).astype(np.float32)
    return (y_prompt, y_sample)
```

```python
import numpy as np
import ml_dtypes
from contextlib import ExitStack

import concourse.bass as bass
import concourse.mybir as mybir
from concourse.alu_op_type import AluOpType as ALU
from concourse.bass_utils import run_bass_kernel_spmd

F32 = mybir.dt.float32
BF16 = mybir.dt.bfloat16
AF = mybir.ActivationFunctionType
AX = mybir.AxisListType

T = 4096
D = 1024
NT = T // 128
DEPTH = 2
NE = 16
FE = 512
EPS = 1e-6
NEG = -1e30
NCORES = 8

ENGS = ("tensor", "vector", "scalar", "gpsimd", "sync")
import os
DBG_PARTS = os.environ.get("DBG_PARTS", "CAB")
EPOCH = 30000


_UID = [0]


class Buf:
    def __init__(self, name):
        self.name = name
        _UID[0] += 1
        self.uid = _UID[0]
        self.writers = {}
        self.readers = {}
        self.dsem = None
        self.dcount = 0
        self.w_is_pe = False


class FW:
    def __init__(self, nc, stack):
        self.nc = nc
        self.stack = stack
        self.q = {e: [] for e in ENGS}
        self.seq = {e: 0 for e in ENGS}
        self.epoch = {e: 0 for e in ENGS}
        self.sems = {e: [self._newsem(f"s_{e}_0")] for e in ENGS}
        self.waited = {e: {} for e in ENGS}
        self.nbuf = 0
        self.ndsem = 0
        self.dsem_bufs = []
        self.dpool = []

    def scope_begin(self):
        return len(self.dsem_bufs)

    def scope_end(self, i):
        for sb in self.dsem_bufs[i:]:
            self.dpool.append((sb.dsem, sb.dcount))
            sb.dsem = None
        del self.dsem_bufs[i:]

    def _newsem(self, name):
        return self.stack.enter_context(self.nc.semaphore(name))

    def buf(self, name=None):
        self.nbuf += 1
        return Buf(name or f"b{self.nbuf}")

    def _wait(self, eng, dep):
        if dep[0] == "e":
            _, e2, ep, n = dep
            key = ("e", e2)
            val = (ep, n)
            sem = self.sems[e2][ep]
        else:
            _, sem, n, key = dep
            val = (0, n)
        if self.waited[eng].get(key, (-1, 0)) >= val:
            return
        self.waited[eng][key] = val
        self.q[eng].append(lambda e, sem=sem, n=n: e.wait_ge(sem, n))

    def _deps(self, eng, reads, writes, pe_acc=False):
        for b in reads:
            for w in list(b.writers.values()):
                self._wait(eng, w)
        for b in writes:
            if not (pe_acc and b.w_is_pe and eng == "tensor"):
                for w in list(b.writers.values()):
                    self._wait(eng, w)
            for r in list(b.readers.values()):
                self._wait(eng, r)

    def _tick(self, eng):
        if self.seq[eng] >= EPOCH:
            self.epoch[eng] += 1
            self.seq[eng] = 0
            self.sems[eng].append(self._newsem(f"s_{eng}_{self.epoch[eng]}"))
        self.seq[eng] += 1
        ep = self.epoch[eng]
        return ("e", eng, ep, self.seq[eng]), self.sems[eng][ep]

    def _record(self, key, dep, reads, writes, part, is_pe):
        for b in writes:
            if part:
                b.writers[key] = dep
            else:
                b.writers = {key: dep}
                b.readers = {}
            b.w_is_pe = is_pe
        for b in reads:
            b.readers[key] = dep

    def op(self, eng, fn, reads=(), writes=(), pe_acc=False, part=False):
        self._deps(eng, reads, writes, pe_acc)
        dep, sem = self._tick(eng)
        self.q[eng].append(lambda e, fn=fn, sem=sem: fn(e).then_inc(sem, 1))
        self._record(("e", eng), dep, reads, writes, part, eng == "tensor")

    def dma(self, eng, fn, reads=(), writes=(), sem=None, part=False):
        self._deps(eng, reads, writes)
        sb = sem
        if sb.dsem is None:
            if self.dpool:
                sb.dsem, sb.dcount = self.dpool.pop()
            else:
                self.ndsem += 1
                sb.dsem = self._newsem(f"d_{self.ndsem}")
            self.dsem_bufs.append(sb)
        sb.dcount += 16
        key = ("d", sb.uid)
        dep = ("d", sb.dsem, sb.dcount, key)
        hs = sb.dsem
        self.q[eng].append(lambda e, fn=fn, hs=hs: fn(e).then_inc(hs, 16))
        self._record(key, dep, reads, writes, part, False)

    def barrier(self):
        for x in ENGS:
            for y in ENGS:
                if y != x and (self.seq[y] > 0 or self.epoch[y] > 0):
                    self._wait(x, ("e", y, self.epoch[y], self.seq[y]))
            for sb in self.dsem_bufs:
                self._wait(x, ("d", sb.dsem, sb.dcount, ("d", sb.uid)))

    def emit(self):
        with self.nc.Block() as block:
            @block.tensor
            def _(e):
                for f in self.q["tensor"]:
                    f(e)

            @block.vector
            def _(e):
                for f in self.q["vector"]:
                    f(e)

            @block.scalar
            def _(e):
                for f in self.q["scalar"]:
                    f(e)

            @block.gpsimd
            def _(e):
                for f in self.q["gpsimd"]:
                    f(e)

            @block.sync
            def _(e):
                for f in self.q["sync"]:
                    f(e)


class TL:
    def __init__(self, ap, buf):
        self.ap = ap
        self.b = buf


class Arena:
    def __init__(self, nc, stack, fw, nfloat):
        self.t = stack.enter_context(nc.sbuf_tensor("arena", [128, nfloat], F32))
        self.n = nfloat
        self.off = 0
        self.fw = fw

    def mark(self):
        return self.off

    def release(self, m):
        self.off = m

    def _take(self, nf):
        nf = (nf + 7) // 8 * 8
        assert self.off + nf <= self.n, f"arena overflow {self.off}+{nf}>{self.n}"
        o = self.off
        self.off += nf
        return o

    def f32(self, n, name=None):
        o = self._take(n)
        return TL(self.t[:, o:o + n], self.fw.buf(name))

    def bf16(self, n, name=None):
        assert n % 2 == 0
        o = self._take(n // 2)
        return TL(self.t[:, o:o + n // 2].bitcast(BF16), self.fw.buf(name))


def _bf(x):
    return np.asarray(x, np.float32).astype(ml_dtypes.bfloat16).astype(np.float32)


def _b_rows(i):
    r0a = min(max(2 * i - 4, 0), 56)
    r0b = min(max(2 * i + 1 - 4, 0), 56)
    return list(range(r0a // 2, (r0b + 7) // 2 + 1))


def _b_mask(i, j):
    m = np.full((128, 128), NEG, np.float32)
    qc = np.arange(64)
    c0 = np.clip(qc - 8, 0, 48)
    kc = np.arange(64)
    colok = (kc[:, None] >= c0[None, :]) & (kc[:, None] < c0[None, :] + 16)
    for qr in range(2):
        r = 2 * i + qr
        r0 = min(max(r - 4, 0), 56)
        for kr in range(2):
            krow = 2 * j + kr
            if r0 <= krow < r0 + 8:
                blk = np.where(colok, 0.0, NEG).astype(np.float32)
                m[kr * 64:(kr + 1) * 64, qr * 64:(qr + 1) * 64] = blk
    return m


def _consts():
    c = {}
    pos = np.arange(T)
    row = (pos // 64).astype(np.float32)
    col = (pos % 64).astype(np.float32)
    inv = (10000.0 ** (-np.arange(16, dtype=np.float32) / 16)).astype(np.float32)
    ang_r = row[:, None] * inv[None, :]
    ang_c = col[:, None] * inv[None, :]
    cos = np.concatenate([np.cos(ang_r), np.cos(ang_r), np.cos(ang_c), np.cos(ang_c)], axis=1)
    sin = np.concatenate([np.sin(ang_r), np.sin(ang_r), np.sin(ang_c), np.sin(ang_c)], axis=1)
    c["rope"] = np.concatenate([cos, sin], axis=1).astype(np.float32).reshape(NT, 128, 128)
    slopes = np.array([2.0 ** (-8.0 * (n + 1) / 6) for n in range(6)], np.float32)
    k = np.arange(128)[:, None]
    q = np.arange(128)[None, :]
    ab = np.zeros((128, 6, 384), np.float32)
    for oi, o in enumerate((-1, 0, 1)):
        dist = np.abs((q - k) - 128 * o).astype(np.float32)
        for j in range(2):
            for hh in range(3):
                v = -(slopes[3 * j + hh] * dist) * 8.0
                v = np.where(dist <= 128, v, NEG).astype(np.float32)
                ab[:, oi * 2 + j, hh * 128:(hh + 1) * 128] = v
    hi = _bf(ab)
    lo = _bf(np.where(ab <= -1e29, 0.0, ab - hi))
    c["abias"] = np.stack([hi, lo], axis=0).astype(np.float32)
    pats = {}
    plist = []
    pidx = {}
    for i in range(NT):
        for j in _b_rows(i):
            m = _b_mask(i, j)
            key = m.tobytes()
            if key not in pats:
                pats[key] = len(plist)
                plist.append(m)
            pidx[(i, j)] = pats[key]
    c["maskb"] = _bf(np.stack(plist, axis=1))
    c["_pidx"] = pidx
    c["_npat"] = len(plist)
    jp = np.zeros((128, 128), np.float32)
    for qr in range(2):
        for qc in range(64):
            jp[qr * 64 + 63 - qc, qr * 64 + qc] = 1.0
    c["jperm"] = jp
    c["invn3"] = np.tile(np.array([[1 / 384.0, 1 / 256.0, 1 / 384.0]], np.float32), (128, 1))
    return c


_CONST = None


def _get_consts():
    global _CONST
    if _CONST is None:
        _CONST = _consts()
    return _CONST


def build(NSEQ=2, NL=DEPTH, stop_after=None, dbg=False):
    C = _get_consts()
    NPAT = C["_npat"]
    pidx = C["_pidx"]
    nc = bass.Bass("TRN2", target_bir_lowering=False)

    def dram(name, shape, dt=F32, kind="ExternalInput"):
        return nc.dram_tensor(name, list(shape), dt, kind=kind)

    xin = [dram(f"x{s}", [T, D]).ap() for s in range(NSEQ)]
    yout = [dram(f"y{s}", [T, D], kind="ExternalOutput").ap() for s in range(NSEQ)]
    ln1_d = dram("ln1", [DEPTH, 128, 8]).ap()
    w_in_d = dram("w_in", [DEPTH, D, 2048]).ap()
    qkg_d = dram("qk_gain", [DEPTH, 3, 2, 64])
    sink_d = dram("sink", [DEPTH, 6])
    rpb_d = dram("rpb", [DEPTH, 4, 15, 31]).ap()
    og_d = dram("out_gain", [DEPTH, 128, 8]).ap()
    w_out_d = dram("w_out", [DEPTH, D, D]).ap()
    ln2_d = dram("ln2", [DEPTH, 128, 8]).ap()
    wr_d = dram("w_router", [DEPTH, 128, 8, 20]).ap()
    gcol_d = dram("gcolh", [DEPTH, 128, 4]).ap()
    brg_d = dram("b_router_group", [DEPTH, 4])
    bre_d = dram("b_router_expert", [DEPTH, 16])
    wg_d = dram("w_gate", [DEPTH, NE, D, FE]).ap()
    wu_d = dram("w_up", [DEPTH, NE, D, FE]).ap()
    wd_d = dram("w_down", [DEPTH, NE, FE, D]).ap()
    rope_d = dram("c_rope", [NT, 128, 128]).ap()
    abias_d = dram("c_abias", [2, 128, 6, 384]).ap()
    maskb_d = dram("c_maskb", [128, NPAT, 128]).ap()
    jperm_d = dram("c_jperm", [128, 128]).ap()
    invn3_d = dram("c_invn3", [128, 3]).ap()

    skind = "ExternalOutput" if dbg else "Internal"
    x2s = [dram(f"x2s{s}", [T, D], kind=skind).ap() for s in range(NSEQ)]
    xl = [dram(f"xl{s}", [T, D], kind=skind).ap() for s in range(NSEQ)]
    qTs = [dram(f"qTs{s}", [16, 128, T], BF16, kind="Internal").ap() for s in range(NSEQ)]
    PADN = 512 + 1860 + 512
    rpad = dram("rpad", [PADN], kind="Internal")

    with ExitStack() as st:
        fw = FW(nc, st)

        def sb(name, shape, dt=F32):
            t = st.enter_context(nc.sbuf_tensor(name, list(shape), dt))
            return TL(t, fw.buf(name))

        identf = sb("identf", [128, 128], F32)
        identb = sb("identb", [128, 128], BF16)
        jperm = sb("jperm", [128, 128], F32)
        invn3 = sb("invn3", [128, 3], F32)
        abias = sb("abias", [128, 2, 6, 384], BF16)
        maskb = sb("maskb", [128, NPAT, 128], BF16)
        rbt = sb("rbt", [128, 2, 7, 512], BF16)
        g1 = sb("g1", [128, 8]); og = sb("og", [128, 8]); g2 = sb("g2", [128, 8])
        gcol = sb("gcol", [128, 4])
        gainC = sb("gainC", [128, 512])
        esA = sb("esA", [128, 6])
        wr = sb("wr", [128, 8, 20]); rb = sb("rb", [128, 20])
        ztile = sb("ztile", [4, 128])
        gtile = sb("gtile", [128, 128])
        epsb = sb("epsb", [128, 1])
        psall = st.enter_context(nc.psum_tensor("psall", [128, 4096], F32))
        banks = []
        for i in range(8):
            tl_ = TL(psall[:, i * 512:(i + 1) * 512], fw.buf(f"bank{i}"))
            tl_.col0 = i * 512
            banks.append(tl_)
        scrbuf = fw.buf("dram_scratch")
        outbuf = fw.buf("dram_out")
        arena = Arena(nc, st, fw, 43200)

        def V(tl):
            return tl.ap if isinstance(tl, TL) else tl

        def act(out, in_, func, reads, writes, part=False, **kw):
            fw.op("scalar", lambda e: e.activation(out=out, in_=in_, func=func, **kw), reads, writes, part=part)

        def rsqrt_inplace(tl, ap, n_inv):
            fw.op("vector", lambda e: e.tensor_scalar(out=ap, in0=ap, scalar1=n_inv, scalar2=EPS,
                                                       op0=ALU.mult, op1=ALU.add), [tl.b], [tl.b])
            act(ap, ap, AF.Ln, [tl.b], [tl.b])
            act(ap, ap, AF.Exp, [tl.b], [tl.b], scale=-0.5)

        fw.op("gpsimd", lambda e: e.memset(identf.ap[:], 0.0), [], [identf.b])
        fw.op("gpsimd", lambda e: e.affine_select(out=identf.ap[:], in_=identf.ap[:], pattern=[[-1, 128]],
                                                   compare_op=ALU.not_equal, fill=1.0, base=0,
                                                   channel_multiplier=1), [identf.b], [identf.b])
        fw.op("vector", lambda e: e.tensor_copy(out=identb.ap[:], in_=identf.ap[:]), [identf.b], [identb.b])
        fw.op("vector", lambda e: e.memset(ztile.ap[:], 0.0), [], [ztile.b])
        fw.dma("sync", lambda e: e.dma_start(out=jperm.ap[:], in_=jperm_d[:, :]), [], [jperm.b], sem=jperm.b)
        fw.dma("sync", lambda e: e.dma_start(out=invn3.ap[:], in_=invn3_d[:, :]), [], [invn3.b], sem=invn3.b)
        fw.dma("gpsimd", lambda e: e.dma_start(out=abias.ap[:], in_=abias_d.rearrange("t k a c -> k t a c")),
               [], [abias.b], sem=abias.b)
        fw.dma("gpsimd", lambda e: e.dma_start(out=maskb.ap[:], in_=maskb_d[:, :, :]), [], [maskb.b], sem=maskb.b)
        rpa = rpad.ap()
        fw.dma("sync", lambda e: e.dma_start(out=rpa[0:512].rearrange("(a b) -> a b", b=128), in_=ztile.ap[:]),
               [ztile.b], [scrbuf], sem=ztile.b, part=True)
        fw.dma("sync", lambda e: e.dma_start(out=rpa[2372:2884].rearrange("(a b) -> a b", b=128), in_=ztile.ap[:]),
               [ztile.b], [scrbuf], sem=ztile.b, part=True)

        def layer_setup(l):
            fw.dma("sync", lambda e: e.dma_start(out=g1.ap[:], in_=ln1_d[l]), [], [g1.b], sem=g1.b)
            fw.dma("sync", lambda e: e.dma_start(out=og.ap[:], in_=og_d[l]), [], [og.b], sem=og.b)
            fw.dma("sync", lambda e: e.dma_start(out=g2.ap[:], in_=ln2_d[l]), [], [g2.b], sem=g2.b)
            fw.dma("sync", lambda e: e.dma_start(out=gcol.ap[:], in_=gcol_d[l]), [], [gcol.b], sem=gcol.b)
            srcq = bass.AP(tensor=qkg_d, offset=((l * 3 + 2) * 2 + 0) * 64, ap=[[0, 128], [0, 6], [1, 64]])
            srck = bass.AP(tensor=qkg_d, offset=((l * 3 + 2) * 2 + 1) * 64, ap=[[0, 128], [0, 2], [1, 64]])
            fw.dma("sync", lambda e: e.dma_start(out=gainC.ap[:, 0:384].rearrange("p (h j) -> p h j", j=64), in_=srcq),
                   [], [gainC.b], sem=gainC.b, part=True)
            fw.dma("sync", lambda e: e.dma_start(out=gainC.ap[:, 384:512].rearrange("p (h j) -> p h j", j=64), in_=srck),
                   [], [gainC.b], sem=gainC.b, part=True)
            srcs = bass.AP(tensor=sink_d, offset=l * 6, ap=[[0, 128], [1, 6]])
            fw.dma("sync", lambda e: e.dma_start(out=esA.ap[:], in_=srcs), [], [esA.b], sem=esA.b)
            act(esA.ap[:], esA.ap[:], AF.Exp, [esA.b], [esA.b])
            fw.dma("sync", lambda e: e.dma_start(out=wr.ap[:], in_=wr_d[l]), [], [wr.b], sem=wr.b)
            s1 = bass.AP(tensor=brg_d, offset=l * 4, ap=[[0, 128], [1, 4]])
            s2 = bass.AP(tensor=bre_d, offset=l * 16, ap=[[0, 128], [1, 16]])
            fw.dma("sync", lambda e: e.dma_start(out=rb.ap[:, 0:4], in_=s1), [], [rb.b], sem=rb.b, part=True)
            fw.dma("sync", lambda e: e.dma_start(out=rb.ap[:, 4:20], in_=s2), [], [rb.b], sem=rb.b, part=True)
            fw.dma("sync", lambda e: e.dma_start(out=rpa[512:2372].rearrange("(a b) -> a b", b=465),
                                                   in_=rpb_d[l].rearrange("h a b -> h (a b)")),
                   [], [scrbuf], sem=scrbuf, part=True)
            fw.barrier()
            mrb = arena.mark()
            scrb = fw.scope_begin()
            gall = arena.f32(28 * 128, "gall")
            for h in range(4):
                for oi in range(7):
                    o = oi - 3
                    gi = h * 7 + oi
                    for qr in range(2):
                        src = bass.AP(tensor=rpad, offset=512 + h * 465 + (2 * o + 7 - qr) * 31 + 15 - 63,
                                      ap=[[1, 64], [31, 2], [1, 64]])
                        qn = ("sync", "scalar")[(gi * 2 + qr) % 2]
                        fw.dma(qn, lambda e, src=src, qr=qr, gi=gi: e.dma_start(
                            out=gall.ap[qr * 64:(qr + 1) * 64, gi * 128:(gi + 1) * 128].rearrange("p (a b) -> p a b", a=2), in_=src),
                            [], [gall.b], sem=gall.b, part=True)
            for h in range(4):
                for oi in range(7):
                    gi = h * 7 + oi
                    bk = banks[gi % 2]
                    fw.op("tensor", lambda e, bk=bk, gi=gi: e.matmul(bk.ap[:, 0:128], lhsT=gall.ap[:, gi * 128:(gi + 1) * 128],
                                                                     rhs=jperm.ap[:], start=True, stop=True),
                          [gall.b, jperm.b], [bk.b])
                    hi = rbt.ap[:, 0, oi, h * 128:(h + 1) * 128]
                    lo = rbt.ap[:, 1, oi, h * 128:(h + 1) * 128]
                    fw.op("vector", lambda e, hi=hi, bk=bk: e.tensor_scalar(out=hi, in0=bk.ap[:, 0:128], scalar1=8.0,
                                                                             scalar2=None, op0=ALU.mult),
                          [bk.b], [rbt.b], part=True)
                    fw.op("vector", lambda e, hi=hi, lo=lo, bk=bk: e.scalar_tensor_tensor(
                        out=lo, in0=bk.ap[:, 0:128], scalar=8.0, in1=hi, op0=ALU.mult, op1=ALU.subtract),
                        [bk.b, rbt.b], [rbt.b], part=True)
            fw.barrier()
            fw.scope_end(scrb)
            arena.release(mrb)


        def load_w_cast(dst_tl, segs, src2d, nch):
            for (dc, sap, n) in segs:
                fw.dma("gpsimd", lambda e, dc=dc, sap=sap, n=n: e.dma_start(
                    out=dst_tl.ap[:, :, dc:dc + n], in_=sap), [], [dst_tl.b], sem=dst_tl.b, part=True)

        def phase_p1(s, l, KT, VA, xsrc, wout_hook):
            m0 = arena.mark()
            sc0 = fw.scope_begin()
            Win = arena.bf16(8 * 2048, "Win"); Win.ap = Win.ap.rearrange("p (c n) -> p c n", c=8)
            wsrc = w_in_d[l]

            def rows(ap):
                return ap.rearrange("(c p) n -> p c n", p=128)

            wchunk = [fw.buf(f"winc{c}") for c in range(8)]
            for c in range(8):
                fw.dma("gpsimd", lambda e, c=c: e.dma_start(out=Win.ap[:, c, :], in_=wsrc[c * 128:(c + 1) * 128, :]),
                       [], [wchunk[c]], sem=wchunk[c])
            for c in range(8):
                eng = "vector"
                fw.op(eng, lambda e, c=c: e.tensor_scalar(out=Win.ap[:, c, :], in0=Win.ap[:, c, :],
                                                           scalar1=g1.ap[:, c:c + 1], scalar2=None, op0=ALU.mult),
                      [wchunk[c], g1.b], [wchunk[c], Win.b], part=True)
            wout_hook()
            xt = [arena.f32(1024, f"xt{i}") for i in range(3)]
            rt = [arena.f32(128, f"rt{i}") for i in range(2)]
            junk = arena.f32(1536, "junk")
            sqtok = [fw.buf(f"sqtok{g}") for g in range(3)]
            junkx = arena.bf16(1024, "junkx")
            raw = arena.f32(1536, "raw")
            hb = [arena.bf16(1024, f"hb{i}") for i in range(2)]
            hT = [arena.bf16(1024, f"hT{i}") for i in range(2)]
            ssx = [arena.f32(8, f"ssx{i}") for i in range(2)]
            ssh = [arena.f32(24, f"ssh{i}") for i in range(2)]
            qkab = [arena.bf16(1024, f"qkab{i}") for i in range(2)]
            qc32 = arena.f32(512, "qc32"); t1 = arena.f32(512, "t1"); t2 = arena.f32(512, "t2")
            qcb = [arena.bf16(512, f"qcb{i}") for i in range(2)]
            qst = [arena.bf16(16 * 128, f"qst{i}") for i in range(2)]
            for q_ in qst:
                fw.op("gpsimd", lambda e, q_=q_: e.memset(q_.ap[:], 0.0), [], [q_.b])
            bT = [banks[0], banks[1]]
            bP = banks[2:6]
            bQ = banks[6]; bR = banks[7]
            bQb = bQ.ap[:, :].bitcast(BF16); bRb = bR.ap[:, :].bitcast(BF16)

            def xload(t):
                x_ = xt[t % 3]
                fw.dma("sync", lambda e, x_=x_, t=t: e.dma_start(out=x_.ap[:], in_=xsrc[t * 128:(t + 1) * 128, :]),
                       [], [x_.b], sem=x_.b)

            def rload(t):
                r_ = rt[t % 2]
                fw.dma("sync", lambda e, r_=r_, t=t: e.dma_start(out=r_.ap[:], in_=rope_d[t]), [], [r_.b], sem=r_.b)

            def stage0a(t):
                i = t % 2
                x_ = xt[t % 3]; sx = ssx[i]
                act(junkx.ap[:], x_.ap[:], AF.Square, [x_.b], [junkx.b, sx.b], accum_out=sx.ap[:, 0:1])
                rsqrt_inplace(sx, sx.ap[:, 0:1], 1.0 / D)
                h_ = hb[i]
                fw.op("vector", lambda e, h_=h_, x_=x_, sx=sx: e.tensor_scalar(out=h_.ap[:], in0=x_.ap[:], scalar1=sx.ap[:, 0:1],
                                                                                scalar2=None, op0=ALU.mult),
                      [x_.b, sx.b], [h_.b])

            def stage0b(t):
                i = t % 2
                h_ = hb[i]
                bt = bT[i]
                btb = bt.ap[:, :].bitcast(BF16)
                for c in range(8):
                    fw.op("tensor", lambda e, c=c, h_=h_, btb=btb: e.transpose(
                        out=btb[:, c * 128:(c + 1) * 128], in_=h_.ap[:, c * 128:(c + 1) * 128], identity=identb.ap[:]),
                        [h_.b, identb.b], [bt.b], pe_acc=True)
                hT_ = hT[i]
                act(hT_.ap[:], btb[:, 0:1024], AF.Copy, [bt.b], [hT_.b])

            def stage1(t):
                i = t % 2
                hT_ = hT[i]
                for g in range(4):
                    for c in range(8):
                        fw.op("tensor", lambda e, g=g, c=c, hT_=hT_: e.matmul(
                            bP[g].ap[:, :], lhsT=hT_.ap[:, c * 128:(c + 1) * 128], rhs=Win.ap[:, c, g * 512:(g + 1) * 512],
                            start=(c == 0), stop=(c == 7)), [hT_.b, Win.b], [bP[g].b], pe_acc=True)
                for g in range(3):
                    act(junk.ap[:, g * 512:(g + 1) * 512], bP[g].ap[:, :], AF.Square, [bP[g].b], [junk.b, sqtok[g]], part=True)
                    fw.op("vector", lambda e, g=g: e.tensor_copy(out=raw.ap[:, g * 512:(g + 1) * 512], in_=bP[g].ap[:, :]),
                          [bP[g].b, sqtok[g]], [raw.b], part=True)
                act(VA.ap[:, t, :, 0:64], bP[3].ap[:, :].rearrange("p (h j) -> p h j", j=64), AF.Copy, [bP[3].b], [VA.b],
                    part=True)

            def stage2(t):
                i = t % 2
                r_ = rt[i]; sh = ssh[i]
                fw.op("vector", lambda e, sh=sh: e.reduce_sum(out=sh.ap[:, 0:24], in_=junk.ap[:].rearrange("p (h j) -> p h j", j=64),
                                                               axis=AX.X), [junk.b], [sh.b])
                rsqrt_inplace(sh, sh.ap[:, 0:24], 1.0 / 64)
                qk_ = qkab[i]
                fw.op("vector", lambda e, qk_=qk_, sh=sh: e.tensor_tensor(
                    out=qk_.ap[:].rearrange("p (h j) -> p h j", j=64),
                    in0=raw.ap[:, 0:1024].rearrange("p (h j) -> p h j", j=64),
                    in1=sh.ap[:, 0:16].unsqueeze(2).to_broadcast([128, 16, 64]), op=ALU.mult),
                    [raw.b, sh.b], [qk_.b])
                fw.op("vector", lambda e, sh=sh: e.tensor_tensor(
                    out=qc32.ap[:].rearrange("p (h j) -> p h j", j=64),
                    in0=raw.ap[:, 1024:1536].rearrange("p (h j) -> p h j", j=64),
                    in1=sh.ap[:, 16:24].unsqueeze(2).to_broadcast([128, 8, 64]), op=ALU.mult),
                    [raw.b, sh.b], [qc32.b])
                fw.op("gpsimd", lambda e: e.tensor_tensor(out=qc32.ap[:], in0=qc32.ap[:], in1=gainC.ap[:], op=ALU.mult),
                      [qc32.b, gainC.b], [qc32.b])
                v5 = lambda ap: ap.rearrange("p (h a w f) -> p h a w f", h=8, a=2, w=2)
                cosb = r_.ap[:, 0:64].unsqueeze(1).to_broadcast([128, 8, 64])
                fw.op("gpsimd", lambda e, cosb=cosb: e.tensor_tensor(out=t1.ap[:].rearrange("p (h j) -> p h j", j=64),
                                                                      in0=qc32.ap[:].rearrange("p (h j) -> p h j", j=64),
                                                                      in1=cosb, op=ALU.mult), [qc32.b, r_.b], [t1.b])
                sin4 = r_.ap[:, 64:128].rearrange("p (a w f) -> p a w f", a=2, w=2)
                q5 = v5(qc32.ap[:]); t25 = v5(t2.ap[:]); t15 = v5(t1.ap[:])
                qcb_ = qcb[i]
                o5 = v5(qcb_.ap[:])
                for w in range(2):
                    sw = sin4[:, :, w, :].unsqueeze(1).to_broadcast([128, 8, 2, 16])
                    fw.op("gpsimd", lambda e, w=w, sw=sw: e.tensor_tensor(out=t25[:, :, :, w, :], in0=q5[:, :, :, 1 - w, :], in1=sw,
                                                                           op=ALU.mult), [qc32.b, r_.b], [t2.b], part=(w > 0))
                fw.op("gpsimd", lambda e, o5=o5: e.tensor_tensor(out=o5[:, :, :, 0, :], in0=t15[:, :, :, 0, :],
                                                                  in1=t25[:, :, :, 0, :], op=ALU.subtract),
                      [t1.b, t2.b], [qcb_.b], part=True)
                fw.op("gpsimd", lambda e, o5=o5: e.tensor_tensor(out=o5[:, :, :, 1, :], in0=t15[:, :, :, 1, :],
                                                                  in1=t25[:, :, :, 1, :], op=ALU.add),
                      [t1.b, t2.b], [qcb_.b], part=True)

            def stage3(t):
                i = t % 2
                qk_ = qkab[i]; qcb_ = qcb[i]
                for b in range(8):
                    fw.op("tensor", lambda e, b=b, qk_=qk_: e.transpose(out=bQb[:, b * 128:(b + 1) * 128],
                                                                        in_=qk_.ap[:, b * 128:(b + 1) * 128],
                                                                        identity=identb.ap[:]),
                          [qk_.b, identb.b], [bQ.b], pe_acc=True)
                for b in range(4):
                    fw.op("tensor", lambda e, b=b, qcb_=qcb_: e.transpose(out=bRb[:, b * 128:(b + 1) * 128],
                                                                          in_=qcb_.ap[:, b * 128:(b + 1) * 128],
                                                                          identity=identb.ap[:]),
                          [qcb_.b, identb.b], [bR.b], pe_acc=True)
                q_ = qst[i]
                q4 = q_.ap[:].rearrange("p (b v t) -> p b v t", b=8, v=2)
                tok = slice(t * 128, (t + 1) * 128)
                for v in range(2):
                    ps = slice(v * 64, (v + 1) * 64)
                    act(q4[ps, 0:3, v, :], bQb[ps, 0:384].rearrange("p (b t) -> p b t", b=3), AF.Copy, [bQ.b, gcol.b], [q_.b],
                        part=True, scale=gcol.ap[ps, 0:1])
                    act(q4[ps, 3:5, v, :], bQb[ps, 512:768].rearrange("p (b t) -> p b t", b=2), AF.Copy, [bQ.b, gcol.b], [q_.b],
                        part=True, scale=gcol.ap[ps, 2:3])
                    fw.op("vector", lambda e, v=v, ps=ps, q4=q4: e.tensor_copy(
                        out=q4[ps, 5:8, v, :], in_=bRb[ps, 0:384].rearrange("p (b t) -> p b t", b=3)), [bR.b], [q_.b], part=True)
                act(KT.ap[:, 0, tok], bQb[:, 384:512], AF.Copy, [bQ.b, gcol.b], [], scale=gcol.ap[:, 1:2])
                act(KT.ap[:, 1:3, tok], bQb[:, 768:1024].rearrange("p (b t) -> p b t", b=2), AF.Copy, [bQ.b, gcol.b], [],
                    scale=gcol.ap[:, 3:4])
                fw.op("vector", lambda e, tok=tok: e.tensor_copy(out=KT.ap[:, 3, tok], in_=bRb[:, 384:512]), [bR.b], [])
                fw.dma("sync", lambda e, q_=q_, tok=tok: e.dma_start(
                    out=qTs[s][:, :, tok].rearrange("b p t -> p b t"), in_=q_.ap[:].rearrange("p (b t) -> p b t", b=16)),
                    [q_.b], [scrbuf], sem=q_.b, part=True)

            xload(0); xload(1); xload(2)
            rload(0); rload(1)
            stage0a(0); xload(3); stage0a(1); xload(4)
            stage0b(0); stage0a(2); stage0b(1); stage1(0); stage0a(3); stage2(0)
            for t in range(NT):
                if t + 2 < NT:
                    stage0b(t + 2)
                if t + 1 < NT:
                    stage1(t + 1)
                    stage2(t + 1)
                stage3(t)
                if t + 5 < NT:
                    xload(t + 5)
                if t + 4 < NT:
                    stage0a(t + 4)
                if t + 2 < NT:
                    rload(t + 2)
            fw.barrier()
            fw.scope_end(sc0)
            arena.release(m0)

        def phase_att(s, l, KT, VA, xsrc, Wout):
            m0 = arena.mark()
            sc0 = fw.scope_begin()
            QT = [arena.bf16(16 * 512, f"QT{i}") for i in range(2)]
            for q in QT:
                q.ap = q.ap.rearrange("p (b t) -> p b t", b=16)
            NPT = 3
            PT = [arena.bf16(1536, f"PT{i}") for i in range(NPT)]
            oall = arena.f32(4 * 1024, "oall")
            oall4 = oall.ap[:].rearrange("p (t h j) -> p t h j", t=4, h=16)
            onb = [arena.bf16(1024, f"onb{i}") for i in range(2)]
            onT = [arena.bf16(1024, f"onT{i}") for i in range(2)]
            xt = [arena.f32(1024, f"axt{i}") for i in range(4)]
            junk = arena.f32(1024, "ajunk")
            ss3 = [arena.f32(8, f"ss3{i}") for i in range(2)]
            rden = arena.f32(16, "rden")
            sslots = [(banks[0:3], fw.buf("ss0")), (banks[3:6], fw.buf("ss1"))]
            ob = [banks[6], banks[7]]
            state = {"s": 0, "p": 0}
            from collections import deque
            fillers = deque()

            def next_s():
                b = sslots[state["s"] % 2]
                state["s"] += 1
                return b

            def next_p():
                b = PT[state["p"] % NPT]
                state["p"] += 1
                return b

            def slot_ap(slot, a, b):
                return psall[:, slot[0][0].col0 + a:slot[0][0].col0 + b]

            def run_units(units, L=2):
                n = len(units)
                pts = [None] * n
                for k in range(n + L):
                    if k < n:
                        slot = next_s()
                        pt = next_p()
                        ncols = units[k]["qk"](slot)
                        act(pt.ap[:, 0:ncols], slot_ap(slot, 0, ncols), AF.Exp, [slot[1]], [pt.b], scale=0.125)
                        pts[k] = pt
                    if k - L >= 0:
                        units[k - L]["pv"](pts[k - L])
                    if fillers:
                        fillers.popleft()()

            def mm(out, lhsT, rhs, start, stop, reads, wbuf):
                fw.op("tensor", lambda e: e.matmul(out, lhsT=lhsT, rhs=rhs, start=start, stop=stop,
                                                    skip_group_check=True), reads, [wbuf], pe_acc=True)

            def q_load(qc):
                Q = QT[qc % 2]
                fw.dma("sync", lambda e, Q=Q, qc=qc: e.dma_start(
                    out=Q.ap[:], in_=qTs[s][:, :, qc * 512:(qc + 1) * 512].rearrange("b p t -> p b t")),
                    [], [Q.b], sem=Q.b)

            def x_loads(qc):
                for qt in range(4):
                    t = qc * 4 + qt
                    x_ = xt[qt]
                    fw.dma("sync", lambda e, x_=x_, t=t: e.dma_start(out=x_.ap[:], in_=xsrc[t * 128:(t + 1) * 128, :]),
                           [], [x_.b], sem=x_.b)

            def evacuate(kind, idx):
                for bi in range(2):
                    bk = ob[bi]
                    if kind == "B":
                        qt0 = idx * 2 + bi
                        src = bk.ap[:, 0:260].rearrange("p (g c) -> p g c", c=65)
                        dst = oall4[:, qt0, 6:10, :]
                        rd = rden.ap[:, 0:4]
                        fw.op("vector", lambda e, src=src, rd=rd: e.reciprocal(out=rd, in_=src[:, :, 64]),
                              [bk.b], [rden.b])
                        fw.op("vector", lambda e, src=src, dst=dst, rd=rd: e.tensor_tensor(
                            out=dst, in0=src[:, :, 0:64], in1=rd.unsqueeze(2).to_broadcast([128, 4, 64]), op=ALU.mult),
                            [bk.b, rden.b], [oall.b], part=True)
                    else:
                        hbase = (0 if kind == "A" else 10) + 3 * idx
                        src = bk.ap[:, 0:390].rearrange("p (g c) -> p g c", c=65)
                        rd = rden.ap[:, 0:6]
                        if kind == "A":
                            es = esA.ap[:, 3 * idx:3 * idx + 3].unsqueeze(1).to_broadcast([128, 2, 3])
                            fw.op("vector", lambda e, src=src, rd=rd, es=es: e.tensor_tensor(
                                out=rd.rearrange("p (a b) -> p a b", a=2),
                                in0=src[:, :, 64].rearrange("p (a b) -> p a b", a=2), in1=es, op=ALU.add),
                                [bk.b, esA.b], [rden.b])
                            fw.op("vector", lambda e, rd=rd: e.reciprocal(out=rd, in_=rd), [rden.b], [rden.b])
                        else:
                            fw.op("vector", lambda e, src=src, rd=rd: e.reciprocal(out=rd, in_=src[:, :, 64]),
                                  [bk.b], [rden.b])
                        dst = oall4[:, bi * 2:bi * 2 + 2, hbase:hbase + 3, :]
                        fw.op("vector", lambda e, src=src, dst=dst, rd=rd: e.tensor_tensor(
                            out=dst, in0=src[:, :, 0:64].rearrange("p (a b) c -> p a b c", a=2),
                            in1=rd.rearrange("p (a b) -> p a b", a=2).unsqueeze(3).to_broadcast([128, 2, 3, 64]), op=ALU.mult),
                            [bk.b, rden.b], [oall.b], part=True)

            def finalize_steps(qc):
                steps = []
                for qt in range(4):
                    t = qc * 4 + qt
                    i2 = t % 2
                    x_ = xt[qt]
                    o_ = oall.ap[:, qt * 1024:(qt + 1) * 1024]
                    segs = [(0, 384), (384, 640), (640, 1024)]
                    s3 = ss3[i2]
                    on_ = onb[i2]
                    oT_ = onT[i2]

                    def st_a(o_=o_, s3=s3):
                        for gi, (a, b) in enumerate(segs):
                            act(junk.ap[:, a:b], o_[:, a:b], AF.Square, [oall.b], [junk.b, s3.b], part=(gi > 0),
                                accum_out=s3.ap[:, gi:gi + 1])

                    def st_b(s3=s3):
                        fw.op("vector", lambda e: e.tensor_tensor(out=s3.ap[:, 0:3], in0=s3.ap[:, 0:3], in1=invn3.ap[:],
                                                                   op=ALU.mult), [s3.b, invn3.b], [s3.b])
                        rsqrt_inplace(s3, s3.ap[:, 0:3], 1.0)

                    def st_c(o_=o_, s3=s3, on_=on_):
                        for gi, (a, b) in enumerate(segs):
                            fw.op("vector", lambda e, a=a, b=b, gi=gi: e.tensor_scalar(
                                out=on_.ap[:, a:b], in0=o_[:, a:b], scalar1=s3.ap[:, gi:gi + 1], scalar2=None, op0=ALU.mult),
                                [oall.b, s3.b], [on_.b], part=(gi > 0))

                    box = {}

                    def st_d(on_=on_, oT_=oT_, box=box):
                        slot = next_s()
                        box["slot"] = slot
                        btb = slot[0][0].ap[:, :].bitcast(BF16)
                        for c in range(8):
                            fw.op("tensor", lambda e, c=c: e.transpose(
                                out=btb[:, c * 128:(c + 1) * 128], in_=on_.ap[:, c * 128:(c + 1) * 128], identity=identb.ap[:]),
                                [on_.b, identb.b], [slot[1]], pe_acc=True)
                        fw.op("vector", lambda e: e.tensor_copy(out=oT_.ap[:], in_=btb[:, 0:1024]), [slot[1]], [oT_.b])

                    def st_e(hf, oT_=oT_, x_=x_, t=t):
                        slot = next_s()
                        by = slot[0][0]
                        for c in range(8):
                            fw.op("tensor", lambda e, c=c: e.matmul(
                                by.ap[:, :], lhsT=oT_.ap[:, c * 128:(c + 1) * 128], rhs=Wout.ap[:, c, hf * 512:(hf + 1) * 512],
                                start=(c == 0), stop=(c == 7)), [oT_.b, Wout.b], [slot[1]], pe_acc=True)
                        fw.op("vector", lambda e: e.tensor_tensor(
                            out=x_.ap[:, hf * 512:(hf + 1) * 512], in0=by.ap[:, :], in1=x_.ap[:, hf * 512:(hf + 1) * 512],
                            op=ALU.add), [slot[1], x_.b], [x_.b])
                        if hf == 1:
                            fw.dma("sync", lambda e: e.dma_start(out=x2s[s][t * 128:(t + 1) * 128, :], in_=x_.ap[:]),
                                   [x_.b], [scrbuf], sem=x_.b, part=True)

                    st_e0 = lambda st_e=st_e: st_e(0)
                    st_e1 = lambda st_e=st_e: st_e(1)
                    steps.append([st_a, st_b, st_c, st_d, st_e0, st_e1])
                out = []
                for pr_ in range(2):
                    for k in range(6):
                        out += [steps[2 * pr_][k], steps[2 * pr_ + 1][k]]
                        if k < 4:
                            out.append(lambda: None)
                return out

            q_load(0)
            x_loads(0)
            for qc in range(8):
                Q = QT[qc % 2]
                Q4 = Q.ap[:].rearrange("p (b v) t -> p b v t", v=2)
                if qc + 1 < 8:
                    q_load(qc + 1)
                parts = []
                for j in range(2):
                    started = set()
                    units = []
                    for kb in range(NT):
                        def qk(slot, kb=kb, j=j, Q4=Q4):
                            for hh in range(3):
                                bk = slot[0][hh]
                                mm(bk.ap[:, :], KT.ap[:, 3, kb * 128:(kb + 1) * 128], Q4[:, 5 + hh, j, :],
                                   True, True, [Q.b], slot[1])
                            return 1536

                        def pv(pt, kb=kb, j=j, started=started):
                            for hh in range(3):
                                for qt in range(4):
                                    g = qt * 3 + hh
                                    bk = ob[g // 6]
                                    col = (g % 6) * 65
                                    first = bk not in started
                                    started.add(bk)
                                    mm(bk.ap[:, col:col + 65], pt.ap[:, hh * 512 + qt * 128:hh * 512 + (qt + 1) * 128],
                                       VA.ap[:, kb, 6 + j, :], first, kb == NT - 1, [pt.b], bk.b)
                        units.append({"qk": qk, "pv": pv})
                    parts.append(("C", j, units))
                for j in range(2):
                    started = set()
                    units = []
                    for qt in range(4):
                        i = qc * 4 + qt
                        for o in (-1, 0, 1):
                            kb = i + o
                            if kb < 0 or kb >= NT:
                                continue

                            def qk(slot, kb=kb, qt=qt, o=o, j=j, Q4=Q4):
                                bk = slot[0][0]
                                mm(bk.ap[:, 0:384].rearrange("p (h t) -> p h t", h=3),
                                   KT.ap[:, 0, kb * 128:(kb + 1) * 128], Q4[:, 0:3, j, qt * 128:(qt + 1) * 128],
                                   True, False, [Q.b], slot[1])
                                mm(bk.ap[:, 0:384], identb.ap[:], abias.ap[:, 0, (o + 1) * 2 + j, :], False, False,
                                   [abias.b, identb.b], slot[1])
                                mm(bk.ap[:, 0:384], identb.ap[:], abias.ap[:, 1, (o + 1) * 2 + j, :], False, True,
                                   [abias.b, identb.b], slot[1])
                                return 384

                            def pv(pt, kb=kb, qt=qt, j=j, started=started, last=(o == 1 or kb == NT - 1)):
                                for hh in range(3):
                                    g = qt * 3 + hh
                                    bk = ob[g // 6]
                                    col = (g % 6) * 65
                                    first = bk not in started
                                    started.add(bk)
                                    mm(bk.ap[:, col:col + 65], pt.ap[:, hh * 128:(hh + 1) * 128], VA.ap[:, kb, 0 + j, :],
                                       first, last, [pt.b], bk.b)
                            units.append({"qk": qk, "pv": pv})
                    parts.append(("A", j, units))
                for qp in range(2):
                    started = set()
                    units = []
                    for ql in range(2):
                        qt = qp * 2 + ql
                        i = qc * 4 + qt
                        js = _b_rows(i)
                        for kb in js:
                            def qk(slot, kb=kb, qt=qt, i=i, Q4=Q4):
                                bk = slot[0][0]
                                for hbq in range(4):
                                    mm(bk.ap[:, hbq * 128:(hbq + 1) * 128], KT.ap[:, 1 + hbq // 2, kb * 128:(kb + 1) * 128],
                                       Q4[:, 3 + hbq // 2, hbq % 2, qt * 128:(qt + 1) * 128], hbq == 0, False, [Q.b], slot[1])
                                oi = kb - i + 3
                                mm(bk.ap[:, 0:512], identb.ap[:], rbt.ap[:, 0, oi, :], False, False, [identb.b], slot[1])
                                mm(bk.ap[:, 0:512], identb.ap[:], rbt.ap[:, 1, oi, :], False, False, [identb.b], slot[1])
                                mk = maskb.ap[:, pidx[(i, kb)], :]
                                for hbq in range(4):
                                    mm(bk.ap[:, hbq * 128:(hbq + 1) * 128], identb.ap[:], mk, False, hbq == 3, [maskb.b], slot[1])
                                return 512

                            def pv(pt, kb=kb, ql=ql, started=started, last=(kb == js[-1])):
                                for hbq in range(4):
                                    bk = ob[ql]
                                    col = hbq * 65
                                    first = bk not in started
                                    started.add(bk)
                                    mm(bk.ap[:, col:col + 65], pt.ap[:, hbq * 128:(hbq + 1) * 128], VA.ap[:, kb, 2 + hbq, :],
                                       first, last, [pt.b], bk.b)
                            units.append({"qk": qk, "pv": pv})
                    parts.append(("B", qp, units))

                for pi, (kind, idx, units) in enumerate(parts):
                    run_units(units)
                    evacuate(kind, idx)
                while fillers:
                    fillers.popleft()()
                fillers.extend(finalize_steps(qc))
                if qc + 1 < 8:
                    fillers.append(lambda qc=qc: x_loads(qc + 1))
            while fillers:
                fillers.popleft()()
            fw.barrier()
            fw.scope_end(sc0)
            arena.release(m0)

        def phase_moe(s, l, dst):
            from collections import deque
            m0 = arena.mark()
            sc0 = fw.scope_begin()
            ST = 1024
            NSUB = ST // 128
            NS = NSUB
            nst = T // ST
            yacc = arena.f32(NSUB * 1024, "yacc")
            yacc3 = yacc.ap[:].rearrange("p (s d) -> p s d", s=NSUB)
            h2Ts = [arena.bf16(8 * ST, f"h2T{i}") for i in range(2)]
            for h in h2Ts:
                h.ap = h.ap.rearrange("p (c t) -> p c t", c=8)
            gatess = [arena.f32(NSUB * 16, f"gates{i}") for i in range(2)]
            h2f = [arena.f32(1024, f"h2f{i}") for i in range(2)]
            hTf = [arena.f32(1024, f"hTf{i}") for i in range(2)]
            xe = [arena.f32(1024, f"xe{i}") for i in range(4)]
            junk = arena.f32(1024, "mjunk")
            ssx = [arena.f32(8, f"mssx{i}") for i in range(2)]
            lg = arena.f32(NSUB * 20, "lg")
            gt = {n: arena.f32(NSUB * 16, "g_" + n) for n in ("a",)}
            gs = {n: arena.f32(NSUB * 4, "s_" + n) for n in ("gmax", "pg", "ohg", "ex", "esel", "oh1", "es2", "oh2",
                                                             "m1", "m2", "e2", "w1", "w2", "ws")}
            Wg = [arena.bf16(8 * FE, f"Wg{i}") for i in range(2)]
            Wu = [arena.bf16(8 * FE, f"Wu{i}") for i in range(2)]
            Wd = [arena.bf16(4 * D, f"Wd{i}") for i in range(2)]
            for w in Wg + Wu:
                w.ap = w.ap.rearrange("p (c f) -> p c f", c=8)
            for w in Wd:
                w.ap = w.ap.rearrange("p (c d) -> p c d", c=4)
            aT = [arena.bf16(4 * 512, f"aT{i}") for i in range(2)]
            for a in aT:
                a.ap = a.ap.rearrange("p (c t) -> p c t", c=4)
            sg = [arena.f32(512, f"sg{i}") for i in range(2)]
            bGU = [(banks[0], banks[1]), (banks[2], banks[3])]
            bY = [banks[4], banks[5], banks[6], banks[7]]
            cnt = {"gu": 0, "y": 0, "sg": 0}

            def next_y():
                b = bY[cnt["y"] % 4]
                cnt["y"] += 1
                return b

            def load_expert(e_, slot):
                fw.dma("gpsimd", lambda e: e.dma_start(out=Wg[slot].ap[:], in_=wg_d[l, e_].rearrange("(c p) f -> p c f", p=128)),
                       [], [Wg[slot].b], sem=Wg[slot].b)
                fw.dma("gpsimd", lambda e: e.dma_start(out=Wu[slot].ap[:], in_=wu_d[l, e_].rearrange("(c p) f -> p c f", p=128)),
                       [], [Wu[slot].b], sem=Wu[slot].b)
                fw.dma("gpsimd", lambda e: e.dma_start(out=Wd[slot].ap[:], in_=wd_d[l, e_].rearrange("(c p) d -> p c d", p=128)),
                       [], [Wd[slot].b], sem=Wd[slot].b)

            lg3 = lg.ap[:].rearrange("p (s n) -> p s n", s=NS)

            def prologue_steps(sti):
                kb = sti % 2
                h2T = h2Ts[kb]
                gates = gatess[kb]

                def st_i(sub):
                    t = sti * NSUB + sub
                    hf_ = h2f[sub % 2]; sx = ssx[sub % 2]
                    fw.dma("sync", lambda e: e.dma_start(out=hf_.ap[:], in_=x2s[s][t * 128:(t + 1) * 128, :]),
                           [], [hf_.b], sem=hf_.b)
                    act(junk.ap[:], hf_.ap[:], AF.Square, [hf_.b], [junk.b, sx.b], accum_out=sx.ap[:, 0:1])
                    rsqrt_inplace(sx, sx.ap[:, 0:1], 1.0 / D)
                    fw.op("vector", lambda e: e.tensor_scalar(out=hf_.ap[:], in0=hf_.ap[:], scalar1=sx.ap[:, 0:1],
                                                               scalar2=None, op0=ALU.mult), [hf_.b, sx.b], [hf_.b])

                def st_ii(sub):
                    hf_ = h2f[sub % 2]; hT_ = hTf[sub % 2]
                    for hh in range(2):
                        bt = next_y()
                        for c in range(4):
                            cc = hh * 4 + c
                            fw.op("tensor", lambda e, c=c, cc=cc, bt=bt: e.transpose(
                                out=bt.ap[:, c * 128:(c + 1) * 128], in_=hf_.ap[:, cc * 128:(cc + 1) * 128], identity=identf.ap[:]),
                                [hf_.b, identf.b], [bt.b], pe_acc=True)
                        fw.op("vector", lambda e, hh=hh, bt=bt: e.tensor_tensor(
                            out=hT_.ap[:, hh * 512:(hh + 1) * 512].rearrange("p (c t) -> p c t", c=4),
                            in0=bt.ap[:, :].rearrange("p (c t) -> p c t", c=4),
                            in1=g2.ap[:, hh * 4:(hh + 1) * 4].unsqueeze(2).to_broadcast([128, 4, 128]), op=ALU.mult),
                            [bt.b, g2.b], [hT_.b], part=(hh > 0))
                    act(h2T.ap[:, :, sub * 128:(sub + 1) * 128], hT_.ap[:].rearrange("p (c t) -> p c t", c=8), AF.Copy,
                        [hT_.b], [h2T.b], part=True)

                def st_iii(sub):
                    hT_ = hTf[sub % 2]
                    bt = next_y()
                    for c in range(8):
                        fw.op("tensor", lambda e, c=c: e.matmul(
                            bt.ap[:, 0:20], lhsT=hT_.ap[:, c * 128:(c + 1) * 128], rhs=wr.ap[:, c, :],
                            start=(c == 0), stop=(c == 7)), [hT_.b, wr.b], [bt.b], pe_acc=True)
                    fw.op("vector", lambda e: e.tensor_tensor(out=lg3[:, sub, :], in0=bt.ap[:, 0:20], in1=rb.ap[:], op=ALU.add),
                          [bt.b, rb.b], [lg.b], part=True)

                def st_gate():
                    gl = lg3[:, :, 0:4]
                    el = lg3[:, :, 4:20].rearrange("p s (g e) -> p s g e", g=4)

                    def S3(n):
                        return gs[n].ap[:].rearrange("p (s e) -> p s e", s=NS)

                    def S1(n):
                        return gs[n].ap[:, 0:NS]

                    def vop(fn, reads, writes):
                        fw.op("vector", fn, reads, writes)

                    bc4 = lambda ap: ap.unsqueeze(2).to_broadcast([128, NS, 4])
                    vop(lambda e: e.tensor_reduce(out=S1("gmax"), in_=gl, axis=AX.X, op=ALU.max), [lg.b], [gs["gmax"].b])
                    vop(lambda e: e.tensor_tensor(out=S3("ohg"), in0=gl, in1=bc4(S1("gmax")), op=ALU.is_equal),
                        [lg.b, gs["gmax"].b], [gs["ohg"].b])
                    vop(lambda e: e.tensor_tensor(out=S3("ex"), in0=gl, in1=bc4(S1("gmax")), op=ALU.subtract),
                        [lg.b, gs["gmax"].b], [gs["ex"].b])
                    act(S3("ex"), S3("ex"), AF.Exp, [gs["ex"].b], [gs["ex"].b])
                    vop(lambda e: e.reduce_sum(out=S1("pg"), in_=S3("ex"), axis=AX.X), [gs["ex"].b], [gs["pg"].b])
                    vop(lambda e: e.reciprocal(out=S1("pg"), in_=S1("pg")), [gs["pg"].b], [gs["pg"].b])
                    ga4 = gt["a"].ap[:].rearrange("p (s g e) -> p s g e", s=NS, g=4)
                    vop(lambda e: e.tensor_tensor(out=ga4, in0=el, in1=S3("ohg").unsqueeze(3).to_broadcast([128, NS, 4, 4]),
                                                  op=ALU.mult), [lg.b, gs["ohg"].b], [gt["a"].b])
                    vop(lambda e: e.reduce_sum(out=S3("esel"), in_=gt["a"].ap[:].rearrange("p (s g e) -> p s e g", s=NS, g=4),
                                               axis=AX.X), [gt["a"].b], [gs["esel"].b])
                    vop(lambda e: e.tensor_reduce(out=S1("m1"), in_=S3("esel"), axis=AX.X, op=ALU.max), [gs["esel"].b], [gs["m1"].b])
                    vop(lambda e: e.tensor_tensor(out=S3("oh1"), in0=S3("esel"), in1=bc4(S1("m1")), op=ALU.is_equal),
                        [gs["esel"].b, gs["m1"].b], [gs["oh1"].b])
                    vop(lambda e: e.scalar_tensor_tensor(out=S3("es2"), in0=S3("oh1"), scalar=NEG, in1=S3("esel"),
                                                         op0=ALU.mult, op1=ALU.add), [gs["oh1"].b, gs["esel"].b], [gs["es2"].b])
                    vop(lambda e: e.tensor_reduce(out=S1("m2"), in_=S3("es2"), axis=AX.X, op=ALU.max), [gs["es2"].b], [gs["m2"].b])
                    vop(lambda e: e.tensor_tensor(out=S3("oh2"), in0=S3("es2"), in1=bc4(S1("m2")), op=ALU.is_equal),
                        [gs["es2"].b, gs["m2"].b], [gs["oh2"].b])
                    vop(lambda e: e.tensor_tensor(out=S1("e2"), in0=S1("m2"), in1=S1("m1"), op=ALU.subtract),
                        [gs["m1"].b, gs["m2"].b], [gs["e2"].b])
                    act(S1("e2"), S1("e2"), AF.Exp, [gs["e2"].b], [gs["e2"].b])
                    vop(lambda e: e.tensor_scalar(out=S1("w1"), in0=S1("e2"), scalar1=1.0, scalar2=None, op0=ALU.add),
                        [gs["e2"].b], [gs["w1"].b])
                    vop(lambda e: e.reciprocal(out=S1("w1"), in_=S1("w1")), [gs["w1"].b], [gs["w1"].b])
                    vop(lambda e: e.tensor_tensor(out=S1("w1"), in0=S1("w1"), in1=S1("pg"), op=ALU.mult),
                        [gs["w1"].b, gs["pg"].b], [gs["w1"].b])
                    vop(lambda e: e.tensor_tensor(out=S1("w2"), in0=S1("w1"), in1=S1("e2"), op=ALU.mult),
                        [gs["w1"].b, gs["e2"].b], [gs["w2"].b])
                    vop(lambda e: e.tensor_tensor(out=S3("ws"), in0=S3("oh1"), in1=bc4(S1("w1")), op=ALU.mult),
                        [gs["oh1"].b, gs["w1"].b], [gs["ws"].b])
                    vop(lambda e: e.tensor_tensor(out=S3("oh2"), in0=S3("oh2"), in1=bc4(S1("w2")), op=ALU.mult),
                        [gs["oh2"].b, gs["w2"].b], [gs["oh2"].b])
                    vop(lambda e: e.tensor_tensor(out=S3("ws"), in0=S3("ws"), in1=S3("oh2"), op=ALU.add),
                        [gs["ws"].b, gs["oh2"].b], [gs["ws"].b])
                    g4 = gates.ap[:].rearrange("p (s g e) -> p s g e", s=NS, g=4)
                    vop(lambda e: e.tensor_tensor(out=g4, in0=S3("ohg").unsqueeze(3).to_broadcast([128, NS, 4, 4]),
                                                  in1=S3("ws").unsqueeze(2).to_broadcast([128, NS, 4, 4]), op=ALU.mult),
                        [gs["ohg"].b, gs["ws"].b], [gates.b])

                steps = []
                for j in range(NSUB + 2):
                    def pos(j=j):
                        if j < NSUB:
                            st_i(j)
                        if 0 <= j - 1 < NSUB:
                            st_ii(j - 1)
                        if 0 <= j - 2 < NSUB:
                            st_iii(j - 2)
                    steps.append(pos)
                steps.append(st_gate)
                return steps

            xpool = xe + h2f + hTf

            def epi_load(sti, sub):
                t = sti * NSUB + sub
                x_ = xpool[sub]
                fw.dma("sync", lambda e: e.dma_start(out=x_.ap[:], in_=x2s[s][t * 128:(t + 1) * 128, :]), [], [x_.b], sem=x_.b)

            def epilogue(sti):
                for sub in range(NSUB):
                    t = sti * NSUB + sub
                    x_ = xpool[sub]
                    fw.op("vector", lambda e, x_=x_, sub=sub: e.tensor_tensor(out=x_.ap[:], in0=x_.ap[:], in1=yacc3[:, sub, :],
                                                                                op=ALU.add), [x_.b, yacc.b], [x_.b])
                    fw.dma("sync", lambda e, x_=x_, t=t: e.dma_start(out=dst[t * 128:(t + 1) * 128, :], in_=x_.ap[:]),
                           [x_.b], [outbuf], sem=x_.b, part=True)

            def GU(sti, e_, tt, slot):
                h2T = h2Ts[sti % 2]
                wg_, wu_ = Wg[slot], Wu[slot]
                a_ = aT[tt % 2]
                for fc in range(4):
                    bg, bu = bGU[cnt["gu"] % 2]
                    cnt["gu"] += 1
                    for (bk, w_) in ((bg, wg_), (bu, wu_)):
                        for c in range(8):
                            fw.op("tensor", lambda e, bk=bk, w_=w_, c=c, fc=fc: e.matmul(
                                bk.ap[:, :], lhsT=w_.ap[:, c, fc * 128:(fc + 1) * 128],
                                rhs=h2T.ap[:, c, tt * 512:(tt + 1) * 512], start=(c == 0), stop=(c == 7)),
                                [w_.b, h2T.b], [bk.b], pe_acc=True)
                    sg_ = sg[cnt["sg"] % 2]
                    cnt["sg"] += 1
                    act(sg_.ap[:], bg.ap[:, :], AF.Silu, [bg.b], [sg_.b])
                    fw.op("vector", lambda e, fc=fc, sg_=sg_, bu=bu: e.tensor_tensor(
                        out=a_.ap[:, fc, :], in0=bu.ap[:, :], in1=sg_.ap[:], op=ALU.mult),
                        [bu.b, sg_.b], [a_.b], part=(fc > 0))

            def DOWN(sti, e_, tt, slot):
                gates3 = gatess[sti % 2].ap[:].rearrange("p (s n) -> p s n", s=NS)
                wd_ = Wd[slot]
                a_ = aT[tt % 2]
                for sl in range(4):
                    sub = tt * 4 + sl
                    for hf in range(2):
                        by = next_y()
                        for fc in range(4):
                            fw.op("tensor", lambda e, by=by, fc=fc, sl=sl, hf=hf: e.matmul(
                                by.ap[:, :], lhsT=a_.ap[:, fc, sl * 128:(sl + 1) * 128],
                                rhs=wd_.ap[:, fc, hf * 512:(hf + 1) * 512], start=(fc == 0), stop=(fc == 3)),
                                [a_.b, wd_.b], [by.b], pe_acc=True)
                        ya = yacc3[:, sub, hf * 512:(hf + 1) * 512]
                        gsc = gates3[:, sub, e_:e_ + 1]
                        if e_ == 0:
                            fw.op("vector", lambda e, by=by, ya=ya, gsc=gsc: e.tensor_scalar(
                                out=ya, in0=by.ap[:, :], scalar1=gsc, scalar2=None, op0=ALU.mult),
                                [by.b, gatess[sti % 2].b], [yacc.b], part=True)
                        else:
                            fw.op("vector", lambda e, by=by, ya=ya, gsc=gsc: e.scalar_tensor_tensor(
                                out=ya, in0=by.ap[:, :], scalar=gsc, in1=ya, op0=ALU.mult, op1=ALU.add),
                                [by.b, gatess[sti % 2].b, yacc.b], [yacc.b], part=True)

            tiles = [(sti, e_, tt) for sti in range(nst) for e_ in range(NE) for tt in range(ST // 512)]
            fq = deque()
            load_expert(0, 0)
            for st_ in prologue_steps(0):
                st_()
            nload = 1
            slot_of = {}
            prev = None
            for n, (sti, e_, tt) in enumerate(tiles):
                if tt == 0:
                    slot_of[(sti, e_)] = (nload - 1) % 2
                    if e_ == 2 and sti + 1 < nst:
                        fq.extend(prologue_steps(sti + 1))
                    if e_ == NE - 1:
                        for sub in range(NSUB):
                            epi_load(sti, sub)
                slot = slot_of[(sti, e_)]
                GU(sti, e_, tt, slot)
                if prev is not None:
                    DOWN(*prev)
                    if prev[1] == NE - 1 and prev[2] == ST // 512 - 1:
                        epilogue(prev[0])
                if tt == 0:
                    if n + 2 < len(tiles):
                        load_expert((e_ + 1) % NE, nload % 2)
                    nload += 1
                prev = (sti, e_, tt, slot)
                if fq:
                    fq.popleft()()
            DOWN(*prev)
            epilogue(prev[0])
            fw.barrier()
            fw.scope_end(sc0)
            arena.release(m0)

        done = False
        for l in range(NL):
            if done:
                break
            layer_setup(l)
            for s in range(NSEQ):
                xsrc = xin[s] if l == 0 else xl[s]
                ms = arena.mark()
                scs = fw.scope_begin()
                Wout = arena.bf16(8 * 1024, "Wout"); Wout.ap = Wout.ap.rearrange("p (c n) -> p c n", c=8)
                KT = arena.bf16(4 * T, "KT"); KT.ap = KT.ap.rearrange("p (b t) -> p b t", b=4)
                VA = arena.bf16(NT * 8 * 66, "VA"); VA.ap = VA.ap.rearrange("p (t h c) -> p t h c", t=NT, h=8)
                VA.ap = VA.ap[:, :, :, 0:65]
                def wout_hook(l=l, Wout=Wout):
                    fw.dma("gpsimd", lambda e: e.dma_start(
                        out=Wout.ap[:], in_=w_out_d[l].rearrange("(c p) n -> p c n", p=128)), [], [Wout.b], sem=Wout.b)
                    for c in range(8):
                        fw.op("vector", lambda e, c=c: e.tensor_scalar(
                            out=Wout.ap[:, c, :], in0=Wout.ap[:, c, :], scalar1=og.ap[:, c:c + 1], scalar2=None, op0=ALU.mult),
                            [Wout.b, og.b], [Wout.b])
                fw.op("gpsimd", lambda e, VA=VA: e.memset(VA.ap[:, :, :, 64:65], 1.0), [], [VA.b])
                phase_p1(s, l, KT, VA, xsrc, wout_hook)
                if stop_after == ("p1", l, s):
                    done = True
                    break
                phase_att(s, l, KT, VA, xsrc, Wout)
                fw.scope_end(scs)
                arena.release(ms)
                if stop_after == ("att", l, s):
                    done = True
                    break
                dst = yout[s] if l == NL - 1 else xl[s]
                phase_moe(s, l, dst)
        fw.barrier()
        fw.emit()
    return nc


_NC_CACHE = {}


def _prep_shared(ln1, w_in, qk_gain, sink, rpb, out_gain, w_out, ln2, w_router_group, b_router_group,
                 w_router_expert, b_router_expert, w_gate, w_up, w_down):
    C = _get_consts()
    f = lambda a: np.ascontiguousarray(np.asarray(a, dtype=np.float32))
    w_in_f = f(w_in)
    perm = []
    for base in (0, 1408):
        blk = []
        for b_ in range(3):
            blk += list(range(base + b_ * 64, base + b_ * 64 + 64)) + list(range(base + (b_ + 3) * 64, base + (b_ + 3) * 64 + 64))
        perm.append(blk)
    cols = (perm[0] + list(range(384, 512)) + list(range(640, 1152)) + perm[1] + list(range(1792, 1920))
            + list(range(512, 640)) + list(range(1152, 1408)) + list(range(1920, 2048)))
    assert len(cols) == 2048 and len(set(cols)) == 2048
    w_in_p = np.ascontiguousarray(w_in_f[:, :, cols])
    pc = lambda a: np.ascontiguousarray(f(a).reshape(DEPTH, 8, 128).transpose(0, 2, 1))
    w_r = np.concatenate([f(w_router_group), f(w_router_expert)], axis=2)
    w_r = np.ascontiguousarray(w_r.reshape(DEPTH, 8, 128, 20).transpose(0, 2, 1, 3))
    qg = f(qk_gain)
    gcolh = np.stack([np.concatenate([qg[:, m, r, :], qg[:, m, r, :]], axis=1) for m in range(2) for r in range(2)], axis=2)
    shared = {
        "ln1": pc(ln1), "w_in": w_in_p, "qk_gain": qg, "sink": f(sink), "rpb": f(rpb),
        "out_gain": pc(out_gain), "w_out": f(w_out), "ln2": pc(ln2),
        "w_router": w_r, "gcolh": np.ascontiguousarray(gcolh), "b_router_group": f(b_router_group),
        "b_router_expert": f(b_router_expert),
        "w_gate": f(w_gate), "w_up": f(w_up), "w_down": f(w_down),
        "c_rope": C["rope"], "c_abias": C["abias"], "c_maskb": C["maskb"], "c_jperm": C["jperm"],
        "c_invn3": C["invn3"],
    }
    return shared


def kernel(x_prompt, x_sample, ln1, w_in, qk_gain, sink, rpb, out_gain, w_out, ln2,
           w_router_group, b_router_group, w_router_expert, b_router_expert, w_gate, w_up, w_down):
    C = _get_consts()
    f = lambda a: np.ascontiguousarray(np.asarray(a, dtype=np.float32))
    seqs = [f(x_prompt[i]) for i in range(x_prompt.shape[0])] + [f(x_sample[i]) for i in range(x_sample.shape[0])]
    nseq = len(seqs)
    slot0 = list(range(8))
    slot1 = {0: 8, 1: 9, 4: 10, 5: 11}
    zeros = np.zeros((T, D), np.float32)
    if "nc" not in _NC_CACHE:
        _NC_CACHE["nc"] = build()
    nc = _NC_CACHE["nc"]
    shared = _prep_shared(ln1, w_in, qk_gain, sink, rpb, out_gain, w_out, ln2, w_router_group, b_router_group,
                          w_router_expert, b_router_expert, w_gate, w_up, w_down)
    in_maps = []
    for c in range(NCORES):
        m = dict(shared)
        m["x0"] = seqs[slot0[c]]
        m["x1"] = seqs[slot1[c]] if c in slot1 else zeros
        in_maps.append(m)
    res = run_bass_kernel_spmd(nc, in_maps, core_ids=list(range(NCORES)))
    outs = [None] * nseq
    for c in range(NCORES):
        outs[slot0[c]] = res.results[c]["y0"]
        if c in slot1:
            outs[slot1[c]] = res.results[c]["y1"]
    nb = x_prompt.shape[0]
    y_prompt = np.stack(outs[:nb], axis=0).astype(np.float32)
    y_sample = np.stack(outs[nb:], axis=0).astype(np.float32)
    return (y_prompt, y_sample)
```
